# Optimizing a Trainium2 kernel written in Bass

```python
import math
import jax, jax.numpy as jnp
from jax import lax
import numpy as np

D_MODEL = 1024
BATCH = 8
SEQ = 4096
DEPTH = 1

D_HEAD = 64
ROPE_THETA = 10000.0
Q_BLOCK = 128
H_DIFF = 4
DIFF_V = 2 * D_HEAD
N_Q_SWA = 8
N_KV_SWA = 2
GQA = N_Q_SWA // N_KV_SWA
WINDOW = 128
MEM_LEN = 256
H_CROSS = 4
D_CROSS = D_MODEL // H_CROSS
N_GROUPS = 4
E_PER_GROUP = 8
N_EXPERTS = N_GROUPS * E_PER_GROUP
TOP_K_INNER = 2
D_FF_EXPERT = 512
EXPERT_BLOCK = 128
EPS = 1e-6

DQ_W = H_DIFF * 2 * D_HEAD
DK_W = H_DIFF * 2 * D_HEAD
DV_W = H_DIFF * DIFF_V
SQ_W = N_Q_SWA * D_HEAD
SK_W = N_KV_SWA * D_HEAD
SV_W = N_KV_SWA * D_HEAD
IN_W = DQ_W + DK_W + DV_W + SQ_W + SK_W + SV_W
SPLITS = [DQ_W, DQ_W + DK_W, DQ_W + DK_W + DV_W, DQ_W + DK_W + DV_W + SQ_W, DQ_W + DK_W + DV_W + SQ_W + SK_W]
MIX_OUT = H_DIFF * DIFF_V + N_Q_SWA * D_HEAD

kernel_name = "hybrid_diffattn_swa_sink_hmoe_block"


def rmsnorm(x, w):
    xf = x.astype(jnp.float32)
    y = xf * lax.rsqrt(jnp.mean(xf * xf, axis=-1, keepdims=True) + EPS)
    return (y * w.astype(jnp.float32)).astype(x.dtype)


def rope_tables(positions):
    inv_freq = jnp.exp(-math.log(ROPE_THETA) * jnp.arange(0, D_HEAD, 2, dtype=jnp.float32) / D_HEAD)
    ang = positions.astype(jnp.float32)[..., None] * inv_freq
    return jnp.cos(ang), jnp.sin(ang)


def rope(x, cos, sin):
    shp = cos.shape[:2] + (1,) * (x.ndim - 3) + cos.shape[-1:]
    c, s = cos.reshape(shp), sin.reshape(shp)
    xf = x.astype(jnp.float32)
    x1, x2 = xf[..., : D_HEAD // 2], xf[..., D_HEAD // 2:]
    return jnp.concatenate([x1 * c - x2 * s, x2 * c + x1 * s], axis=-1).astype(x.dtype)


def diff_attention(q, k, v, lam, subln_w, lambda_init):
    B, S = q.shape[0], q.shape[1]
    nb = S // Q_BLOCK
    scale = D_HEAD ** -0.5
    qb = q.reshape(B, nb, Q_BLOCK, H_DIFF, 2, D_HEAD).transpose(1, 0, 2, 3, 4, 5)
    starts = jnp.arange(nb, dtype=jnp.int32) * Q_BLOCK
    kpos = jnp.arange(S, dtype=jnp.int32)
    qoff = jnp.arange(Q_BLOCK, dtype=jnp.int32)

    def block(args):
        qblk, start = args
        s = jnp.einsum('bqhcd,bkhcd->bhcqk', qblk, k).astype(jnp.float32) * scale
        causal = kpos[None, :] <= (start + qoff)[:, None]
        s = jnp.where(causal, s, -jnp.inf)
        p = jax.nn.softmax(s, axis=-1)
        pd = p[:, :, 0] - lam * p[:, :, 1]
        return jnp.einsum('bhqk,bkhe->bqhe', pd.astype(v.dtype), v)

    o = lax.map(block, (qb, starts))
    o = o.transpose(1, 0, 2, 3, 4).reshape(B, S, H_DIFF, DIFF_V)
    o = rmsnorm(o, subln_w) * (1.0 - lambda_init)
    return o.reshape(B, S, H_DIFF * DIFF_V)


def sliding_window_sink_attention(q, k, v, sinks):
    B, S = q.shape[0], q.shape[1]
    nb = S // Q_BLOCK
    scale = D_HEAD ** -0.5
    qb = q.reshape(B, nb, Q_BLOCK, N_KV_SWA, GQA, D_HEAD)
    kb = k.reshape(B, nb, Q_BLOCK, N_KV_SWA, D_HEAD)
    vb = v.reshape(B, nb, Q_BLOCK, N_KV_SWA, D_HEAD)
    pad = ((0, 0), (1, 0), (0, 0), (0, 0), (0, 0))
    kk = jnp.concatenate([jnp.pad(kb, pad)[:, :-1], kb], axis=2)
    vv = jnp.concatenate([jnp.pad(vb, pad)[:, :-1], vb], axis=2)
    s = jnp.einsum('bnqhgd,bnkhd->bnhgqk', qb, kk).astype(jnp.float32) * scale
    qi = jnp.arange(Q_BLOCK)[:, None]
    ki = jnp.arange(2 * Q_BLOCK)[None, :]
    rel = Q_BLOCK + qi - ki
    band = (rel >= 0) & (rel < WINDOW)
    has_prev = jnp.arange(nb)[:, None, None] > 0
    valid = band[None] & (has_prev | (ki >= Q_BLOCK)[None])
    s = jnp.where(valid[None, :, None, None], s, -jnp.inf)
    sink = jnp.broadcast_to(sinks.astype(jnp.float32).reshape(N_KV_SWA, GQA)[None, None, :, :, None, None],
                            s.shape[:-1] + (1,))
    p = jax.nn.softmax(jnp.concatenate([s, sink], axis=-1), axis=-1)[..., :-1]
    o = jnp.einsum('bnhgqk,bnkhd->bnqhgd', p.astype(v.dtype), vv)
    return o.reshape(B, S, N_Q_SWA * D_HEAD)


def memory_cross_attention(hn, memn, wq, wkv, wo):
    B, S = hn.shape[0], hn.shape[1]
    M = memn.shape[1]
    q = (hn @ wq).reshape(B, S, H_CROSS, D_CROSS)
    kv = memn @ wkv
    k = kv[..., :D_MODEL].reshape(B, M, H_CROSS, D_CROSS)
    v = kv[..., D_MODEL:].reshape(B, M, H_CROSS, D_CROSS)
    s = jnp.einsum('bshd,bmhd->bhsm', q, k).astype(jnp.float32) * (D_CROSS ** -0.5)
    p = jax.nn.softmax(s, axis=-1)
    o = jnp.einsum('bhsm,bmhd->bshd', p.astype(v.dtype), v).reshape(B, S, D_MODEL)
    return o @ wo


def hierarchical_moe(t, w_group, b_group, w_expert, b_expert, w_gate, w_up, w_down):
    N, D = t.shape
    g_logits = (t @ w_group).astype(jnp.float32) + b_group.astype(jnp.float32)
    g_prob = jax.nn.softmax(g_logits, axis=-1)
    g_p, g_sel = lax.top_k(g_prob, 1)
    e_logits = ((t @ w_expert).astype(jnp.float32) + b_expert.astype(jnp.float32)).reshape(N, N_GROUPS, E_PER_GROUP)
    e_in_group = jnp.take_along_axis(e_logits, g_sel[:, :, None], axis=1)[:, 0]
    e_prob = jax.nn.softmax(e_in_group, axis=-1)
    e_p, e_idx = lax.top_k(e_prob, TOP_K_INNER)
    gates = g_p * (e_p / jnp.sum(e_p, axis=-1, keepdims=True))

    eid = (g_sel * E_PER_GROUP + e_idx).reshape(-1).astype(jnp.int32)
    gate = gates.reshape(-1)
    tok = jnp.repeat(jnp.arange(N, dtype=jnp.int32), TOP_K_INNER)
    A = N * TOP_K_INNER

    order = jnp.argsort(eid)
    e_s, tok_s, gate_s = eid[order], tok[order], gate[order]
    counts = jnp.zeros((N_EXPERTS,), jnp.int32).at[eid].add(1)
    starts = jnp.cumsum(counts) - counts
    padded = (counts + EXPERT_BLOCK - 1) // EXPERT_BLOCK * EXPERT_BLOCK
    pends = jnp.cumsum(padded)
    pstarts = pends - padded
    dest = pstarts[e_s] + (jnp.arange(A, dtype=jnp.int32) - starts[e_s])
    P = -(-(A + N_EXPERTS * (EXPERT_BLOCK - 1)) // EXPERT_BLOCK) * EXPERT_BLOCK
    nblk = P // EXPERT_BLOCK
    slot_tok = jnp.full((P,), N, jnp.int32).at[dest].set(tok_s)
    t_pad = jnp.concatenate([t, jnp.zeros((1, D), t.dtype)], axis=0)
    xs = t_pad[slot_tok].reshape(nblk, EXPERT_BLOCK, D)
    blk_start = jnp.arange(nblk, dtype=jnp.int32) * EXPERT_BLOCK
    blk_e = jnp.minimum(jnp.searchsorted(pends, blk_start, side='right'), N_EXPERTS - 1)

    def expert_rows(args):
        xb, e = args
        hdn = jax.nn.silu(xb @ w_gate[e]) * (xb @ w_up[e])
        return hdn @ w_down[e]

    ys = lax.map(expert_rows, (xs, blk_e)).reshape(P, D)
    y_assign = ys[dest].astype(jnp.float32) * gate_s[:, None]
    out = jax.ops.segment_sum(y_assign, tok_s, num_segments=N)
    return out.astype(t.dtype)


def setup_inputs(seed: int = 0) -> dict:
    key = jax.random.key(seed)
    ks = jax.random.split(key, 32)
    f32 = jnp.float32
    nrm = lambda k, shape, scale: jax.random.normal(k, shape, f32) * scale
    gain = lambda k, shape: 1.0 + 0.02 * jax.random.normal(k, shape, f32)
    L = DEPTH
    x = jax.random.normal(ks[0], (BATCH, SEQ, D_MODEL), f32)
    mem = jax.random.normal(ks[1], (BATCH, MEM_LEN, D_MODEL), f32)
    offsets = jax.random.randint(ks[2], (BATCH, 1), 0, 1024, dtype=jnp.int32)
    positions = (offsets + jnp.arange(SEQ, dtype=jnp.int32)[None, :]).astype(jnp.int32)
    return {
        "x": x,
        "mem": mem,
        "positions": positions,
        "ln_mix_w": gain(ks[3], (L, D_MODEL)),
        "w_in": nrm(ks[4], (L, D_MODEL, IN_W), D_MODEL ** -0.5),
        "lambda_q1": nrm(ks[5], (L, D_HEAD), 0.1),
        "lambda_k1": nrm(ks[6], (L, D_HEAD), 0.1),
        "lambda_q2": nrm(ks[7], (L, D_HEAD), 0.1),
        "lambda_k2": nrm(ks[8], (L, D_HEAD), 0.1),
        "subln_w": gain(ks[9], (L, DIFF_V)),
        "sinks": nrm(ks[10], (L, N_Q_SWA), 0.5),
        "w_out": nrm(ks[11], (L, MIX_OUT, D_MODEL), MIX_OUT ** -0.5),
        "ln_cross_w": gain(ks[12], (L, D_MODEL)),
        "ln_mem_w": gain(ks[13], (L, D_MODEL)),
        "wq_cross": nrm(ks[14], (L, D_MODEL, D_MODEL), D_MODEL ** -0.5),
        "wkv_cross": nrm(ks[15], (L, D_MODEL, 2 * D_MODEL), D_MODEL ** -0.5),
        "wo_cross": nrm(ks[16], (L, D_MODEL, D_MODEL), D_MODEL ** -0.5),
        "ln_moe_w": gain(ks[17], (L, D_MODEL)),
        "w_group": nrm(ks[18], (L, D_MODEL, N_GROUPS), D_MODEL ** -0.5),
        "b_group": nrm(ks[19], (L, N_GROUPS), 0.01),
        "w_expert": nrm(ks[20], (L, D_MODEL, N_EXPERTS), D_MODEL ** -0.5),
        "b_expert": nrm(ks[21], (L, N_EXPERTS), 0.01),
        "w_gate": nrm(ks[22], (L, N_EXPERTS, D_MODEL, D_FF_EXPERT), D_MODEL ** -0.5),
        "w_up": nrm(ks[23], (L, N_EXPERTS, D_MODEL, D_FF_EXPERT), D_MODEL ** -0.5),
        "w_down": nrm(ks[24], (L, N_EXPERTS, D_FF_EXPERT, D_MODEL), D_FF_EXPERT ** -0.5),
        "ln_final_w": gain(ks[25], (D_MODEL,)),
    }


def reference(x, mem, positions, ln_mix_w, w_in, lambda_q1, lambda_k1, lambda_q2, lambda_k2, subln_w, sinks,
              w_out, ln_cross_w, ln_mem_w, wq_cross, wkv_cross, wo_cross, ln_moe_w, w_group, b_group,
              w_expert, b_expert, w_gate, w_up, w_down, ln_final_w):
    B, S, D = x.shape
    cos, sin = rope_tables(positions)
    h = x
    for l in range(DEPTH):
        lambda_init = 0.8 - 0.6 * math.exp(-0.3 * l)
        a = rmsnorm(h, ln_mix_w[l])
        proj = a @ w_in[l]
        dq, dk, dv, sq, sk, sv = jnp.split(proj, SPLITS, axis=-1)
        dq = rope(dq.reshape(B, S, H_DIFF, 2, D_HEAD), cos, sin)
        dk = rope(dk.reshape(B, S, H_DIFF, 2, D_HEAD), cos, sin)
        dv = dv.reshape(B, S, H_DIFF, DIFF_V)
        lam = (jnp.exp(jnp.sum(lambda_q1[l].astype(jnp.float32) * lambda_k1[l].astype(jnp.float32)))
               - jnp.exp(jnp.sum(lambda_q2[l].astype(jnp.float32) * lambda_k2[l].astype(jnp.float32)))
               + lambda_init)
        o_diff = diff_attention(dq, dk, dv, lam, subln_w[l], lambda_init)
        sq = rope(sq.reshape(B, S, N_Q_SWA, D_HEAD), cos, sin)
        sk = rope(sk.reshape(B, S, N_KV_SWA, D_HEAD), cos, sin)
        sv = sv.reshape(B, S, N_KV_SWA, D_HEAD)
        o_swa = sliding_window_sink_attention(sq, sk, sv, sinks[l])
        h = h + jnp.concatenate([o_diff, o_swa], axis=-1) @ w_out[l]
        c = rmsnorm(h, ln_cross_w[l])
        m = rmsnorm(mem, ln_mem_w[l])
        h = h + memory_cross_attention(c, m, wq_cross[l], wkv_cross[l], wo_cross[l])
        f = rmsnorm(h, ln_moe_w[l]).reshape(B * S, D)
        h = h + hierarchical_moe(f, w_group[l], b_group[l], w_expert[l], b_expert[l],
                                 w_gate[l], w_up[l], w_down[l]).reshape(B, S, D)
    return rmsnorm(h, ln_final_w)
```

```python
import math
from contextlib import ExitStack
import numpy as np
import concourse.bass as bass
import concourse.mybir as mybir
from concourse.bass_utils import run_bass_kernel_spmd

F32 = mybir.dt.float32
BF16 = mybir.dt.bfloat16
I32 = mybir.dt.int32
ALU = mybir.AluOpType
ACTF = mybir.ActivationFunctionType
AX = mybir.AxisListType

ENGS = ("sync", "scalar", "vector", "gpsimd", "tensor")

S = 4096
D = 1024
NT = 32
NST = 8
INW = 2304
CAP = 512
NE = 32
UB = 2
US = 128 * UB
NUNIT = (8192 + 32 * (US - 1) + US - 1) // US
PS = NUNIT * US
NEG = -30000.0
EPS = 1e-6
TWO_PI = 2.0 * math.pi
C_ID, C_CM, C_PM, C_LT, C_ON, C_IF, C_IO, C_BS, C_PI, C_END = 0, 128, 256, 384, 512, 640, 672, 704, 800, 802
NBLK = NUNIT


class Sem:
    def __init__(self, h, name):
        self.h = h
        self.n = 0
        self.name = name


class Trk:
    __slots__ = ("w", "r")

    def __init__(self):
        self.w = None
        self.r = []


class Prog:
    def __init__(self, nc, stack):
        self.nc = nc
        self.stack = stack
        self.q = {e: [] for e in ENGS}
        self.sems = []
        self.esem = {e: self.sem("e_" + e) for e in ENGS if e != "sync"}
        self.waited = {e: {} for e in ENGS}

    def sem(self, name):
        h = self.stack.enter_context(self.nc.semaphore(name))
        s = Sem(h, name)
        self.sems.append(s)
        return s

    def sbuf(self, name, shape, dtype):
        return self.stack.enter_context(self.nc.sbuf_tensor(name, list(shape), dtype))

    def psum(self, name, shape, dtype):
        return self.stack.enter_context(self.nc.psum_tensor(name, list(shape), dtype))

    def _waits_for(self, eng, reads, writes):
        evs = []
        for t in reads:
            if t.w is not None:
                evs.append(t.w)
        for t in writes:
            if t.w is not None:
                evs.append(t.w)
            evs.extend(t.r)
        out = {}
        for (s, v, e) in evs:
            if e == eng and eng == "tensor":
                continue
            if self.waited[eng].get(s, 0) >= v:
                continue
            if out.get(s, 0) < v:
                out[s] = v
        for s, v in out.items():
            self.waited[eng][s] = v
        return list(out.items())

    def op(self, eng, fn, reads=(), writes=(), inc=True, sem=None, amt=1):
        waits = self._waits_for(eng, reads, writes)
        s = sem if sem is not None else self.esem[eng]
        if inc:
            s.n += amt
            val = s.n
        else:
            val = s.n + amt
        ev = (s, val, eng if sem is None else "dma")
        for t in reads:
            t.r.append(ev)
            if len(t.r) > 64:
                t.r = t.r[-64:]
        for t in writes:
            t.w = ev
            t.r = []
        sh = s.h

        def emit(e):
            for (ws, wv) in waits:
                e.wait_ge(ws.h, wv)
            ins = fn(e)
            if inc:
                ins.then_inc(sh, amt)
        self.q[eng].append(emit)
        return ev

    def dma(self, eng, out, in_, sem, reads=(), writes=(), **kw):
        return self.op(eng, lambda e: e.dma_start(out=out, in_=in_, **kw),
                       reads=reads, writes=writes, sem=sem, amt=16)

    def wait_events(self, eng, evs):
        ws = []
        for (s, v, e) in evs:
            if self.waited[eng].get(s, 0) >= v:
                continue
            self.waited[eng][s] = v
            ws.append((s, v))

        def emit(e):
            for (s, v) in ws:
                e.wait_ge(s.h, v)
        self.q[eng].append(emit)

    def barrier(self, exclude=()):
        evs = [(s, s.n, "x") for s in self.sems if s.n > 0 and s not in exclude]
        for e in ENGS:
            self.wait_events(e, evs)

    def build(self):
        with self.nc.Block() as block:
            for name in ENGS:
                fns = self.q[name]

                def body(e, fns=fns):
                    for f in fns:
                        f(e)
                getattr(block, name)(body)


class Arena:
    def __init__(self, P, words):
        self.t = P.sbuf("arena", [128, words], F32)
        self.top = 0
        self.words = words
        self.hi = 0

    def alloc(self, shape, dtype=F32):
        n = 1
        for s_ in shape:
            n *= s_
        esz = 4 if dtype in (F32, I32) else 2
        w = (n * esz + 3) // 4
        w = (w + 1) // 2 * 2
        ap = self.t[:, self.top:self.top + w]
        self.top += w
        self.hi = max(self.hi, self.top)
        assert self.top <= self.words, f"SBUF arena overflow {self.top} > {self.words}"
        if dtype != F32:
            ap = ap.bitcast(dtype)[:, 0:n]
        if len(shape) == 2:
            ap = ap.rearrange("p (a b) -> p a b", a=shape[0], b=shape[1])
        elif len(shape) == 3:
            ap = ap.rearrange("p (a b c) -> p a b c", a=shape[0], b=shape[1], c=shape[2])
        return ap

    def mark(self):
        return self.top

    def release(self, m):
        self.top = m


def bc(ap, axis, shape):
    return ap.unsqueeze(axis).to_broadcast(list(shape))


def build_program(nc, debug=None):
    dram = lambda name, shape, dt, kind="ExternalInput": nc.dram_tensor(name, list(shape), dt, kind=kind).ap()
    x = dram("x", [S, D], F32)
    mem = dram("mem", [256, D], F32)
    post = dram("post", [128, NT], I32)
    cst = dram("cst", [128, C_END], F32)
    ln_mix_w = dram("ln_mix_w", [1, D], F32)
    w_in = dram("w_in", [1, D, INW], F32)
    lq1 = dram("lambda_q1", [1, 64], F32)
    lk1 = dram("lambda_k1", [1, 64], F32)
    lq2 = dram("lambda_q2", [1, 64], F32)
    lk2 = dram("lambda_k2", [1, 64], F32)
    subln_w = dram("subln_w", [1, 128], F32)
    sinks = dram("sinks", [1, 8], F32)
    w_out = dram("w_out", [1, D, D], F32)
    ln_cross_w = dram("ln_cross_w", [1, D], F32)
    ln_mem_w = dram("ln_mem_w", [1, D], F32)
    wq_cross = dram("wq_cross", [1, D, D], F32)
    wkv_cross = dram("wkv_cross", [1, D, 2 * D], F32)
    wo_cross = dram("wo_cross", [1, D, D], F32)
    ln_moe_w = dram("ln_moe_w", [1, D], F32)
    w_group = dram("w_group", [1, D, 4], F32)
    b_group = dram("b_group", [1, 4], F32)
    w_expert = dram("w_expert", [1, D, 32], F32)
    b_expert = dram("b_expert", [1, 32], F32)
    w_gate = dram("w_gate", [1, NE, D, 512], F32)
    w_up = dram("w_up", [1, NE, D, 512], F32)
    w_down = dram("w_down", [1, NE, 512, D], F32)
    ln_final_w = dram("ln_final_w", [1, D], F32)
    out = dram("out", [S, D], F32, kind="ExternalOutput")
    hbuf = dram("hbuf", [S, D], F32, kind="Internal")
    xs_d = dram("xs_d", [PS + 128, D], BF16, kind="Internal")
    y_d = dram("y_d", [PS + 128, D], F32, kind="Internal")
    wbf = dram("wbf", [NE * 128, 12288], BF16, kind="Internal")

    with ExitStack() as st:
        P = Prog(nc, st)
        A = Arena(P, 52800)
        pp = [P.psum(f"pp{i}", [128, 1024], F32) for i in range(2)]
        banks = [pp[0][:, 0:512], pp[0][:, 512:1024], pp[1][:, 0:512], pp[1][:, 512:1024]] + [P.psum(f"bank{i}", [128, 512], F32)[:, :] for i in range(4, 8)]
        tbank = [Trk() for _ in range(8)]

        def bk_bf(i):
            return banks[i].bitcast(BF16)

        V = lambda fn, r=(), w=(): P.op("vector", fn, reads=r, writes=w)
        Sc = lambda fn, r=(), w=(): P.op("scalar", fn, reads=r, writes=w)
        G = lambda fn, r=(), w=(): P.op("gpsimd", fn, reads=r, writes=w)
        T = lambda fn, r=(), w=(), inc=True: P.op("tensor", fn, reads=r, writes=w, inc=inc)

        s_dbg = P.sem("s_dbg")
        taps = debug.get("taps", ()) if debug else ()

        def tap(name, ap, trk):
            if name not in taps:
                return
            shp = list(ap.shape)
            dt_ = nc.dram_tensor("dbg_" + name, shp, F32, kind="ExternalOutput").ap()
            P.dma("gpsimd", dt_, ap, s_dbg, reads=[trk])

        cstt = A.alloc([C_END])
        identf = cstt[:, C_ID:C_ID + 128]
        identb = A.alloc([128], BF16)
        cmask4 = A.alloc([512], BF16)
        pmask4 = A.alloc([512], BF16)
        ltrib = A.alloc([128], BF16)
        onesb = A.alloc([128], BF16)
        zerob = A.alloc([512], BF16)
        gates = A.alloc([NT, 2])
        dest = A.alloc([NT * 2], I32)
        blkE = A.alloc([NBLK], I32)
        widx = A.alloc([NBLK], I32)
        t_c = Trk()
        t_gd = Trk()
        s_c = P.sem("s_c")
        P.dma("sync", cstt, cst, s_c, writes=[t_c])
        V(lambda e: e.tensor_copy(out=identb, in_=cstt[:, C_ID:C_ID + 128]), [t_c], [t_c])
        V(lambda e: e.tensor_copy(out=cmask4.rearrange("p (a b) -> p a b", b=128), in_=bc(cstt[:, C_CM:C_CM + 128], 1, [128, 4, 128])), [t_c], [t_c])
        V(lambda e: e.tensor_copy(out=pmask4.rearrange("p (a b) -> p a b", b=128), in_=bc(cstt[:, C_PM:C_PM + 128], 1, [128, 4, 128])), [t_c], [t_c])
        V(lambda e: e.tensor_copy(out=ltrib, in_=cstt[:, C_LT:C_LT + 128]), [t_c], [t_c])
        V(lambda e: e.tensor_copy(out=onesb, in_=cstt[:, C_ON:C_ON + 128]), [t_c], [t_c])
        V(lambda e: e.memset(zerob, 0.0), [], [t_c])
        epsb = A.alloc([2])[:, 0:1]
        V(lambda e: e.memset(epsb, EPS), [], [t_c])
        m_persist = A.mark()

        def rstd_from_ss(ss, rstd, n, trk):
            Sc(lambda e: e.activation(out=rstd, in_=ss, func=ACTF.Sqrt, scale=1.0 / n, bias=epsb), [trk, t_c], [trk])
            V(lambda e: e.reciprocal(out=rstd, in_=rstd), [trk], [trk])

        Win = A.alloc([8, INW], BF16)
        Wout = A.alloc([8, D], BF16)
        KTd = A.alloc([4, S], BF16)
        Vda = A.alloc([NT, 4, 129], BF16)
        KTs = A.alloc([5 * 128], BF16)
        Vsa = A.alloc([5, 2, 65], BF16)
        cosT = A.alloc([NT, 32])
        sinT = A.alloc([NT, 32])
        smalls = A.alloc([512])
        lam_n = smalls[:, 0:1]
        esink = smalls[:, 8:16]
        subw = smalls[:, 128:256]
        t_w = Trk()
        t_kv = Trk()
        t_kvs = Trk()
        t_tab = Trk()
        s_w = P.sem("s_w")
        for hf in range(2):
            P.dma("gpsimd", Win[:, :, hf * 1152:(hf + 1) * 1152],
                  w_in[0][:, hf * 1152:(hf + 1) * 1152].rearrange("(c p) n -> p c n", p=128), s_w, writes=[t_w])
        P.dma("gpsimd", Wout, w_out[0].rearrange("(c p) n -> p c n", p=128), s_w, writes=[t_w])
        s_c3 = P.sem("s_c3")
        lnw8 = smalls[:, 384:392]
        P.dma("sync", lnw8, ln_mix_w.rearrange("o (c p) -> p (o c)", p=128), s_c3, writes=[t_tab], allow_slow_non_contiguous=True)
        for c_ in range(8):
            V(lambda e, c_=c_: e.tensor_scalar(out=Win[:, c_, :], in0=Win[:, c_, :], scalar1=lnw8[:, c_:c_ + 1], scalar2=None, op0=ALU.mult), [t_w, t_tab], [t_w])
        G(lambda e: e.memset(Vda, 1.0), [], [t_kv])
        G(lambda e: e.memset(Vsa, 1.0), [], [t_kvs])
        tm0 = A.mark()
        posi = A.alloc([NT], I32)
        posf = A.alloc([NT])
        ang = A.alloc([NT, 32])
        ki = A.alloc([NT, 32], I32)
        kf = A.alloc([NT, 32])
        rr = A.alloc([NT, 32])
        mm = A.alloc([NT, 32])
        lv = A.alloc([4, 64])
        t_t = Trk()
        s_c2 = P.sem("s_c2")
        P.dma("sync", posi, post, s_c2, writes=[t_t])
        for k_, src in enumerate((lq1, lk1, lq2, lk2)):
            P.dma("sync", lv[:, k_, :], src.partition_broadcast(128), s_c2, writes=[t_t])
        P.dma("sync", smalls[:, 16:24], sinks.partition_broadcast(128), s_c2, writes=[t_t])
        P.dma("sync", smalls[:, 256:384], subln_w.partition_broadcast(128), s_c2, writes=[t_t])
        V(lambda e: e.tensor_copy(out=posf, in_=posi), [t_t], [t_t])
        V(lambda e: e.tensor_tensor(out=ang, in0=bc(posf, 2, [128, NT, 32]), in1=bc(cstt[:, C_IF:C_IF + 32], 1, [128, NT, 32]), op=ALU.mult), [t_t, t_c], [t_t])

        def sin_of(dst, shift):
            V(lambda e: e.tensor_scalar(out=kf, in0=ang, scalar1=shift, scalar2=1.0 / TWO_PI, op0=ALU.add, op1=ALU.mult), [t_t], [t_t])
            V(lambda e: e.tensor_copy(out=ki, in_=kf), [t_t], [t_t])
            V(lambda e: e.tensor_copy(out=kf, in_=ki), [t_t], [t_t])
            V(lambda e: e.scalar_tensor_tensor(out=rr, in0=kf, scalar=-TWO_PI, in1=ang, op0=ALU.mult, op1=ALU.add), [t_t], [t_t])
            if shift != 0.0:
                V(lambda e: e.tensor_scalar(out=rr, in0=rr, scalar1=shift, scalar2=None, op0=ALU.add), [t_t], [t_t])
            V(lambda e: e.tensor_scalar(out=mm, in0=rr, scalar1=math.pi, scalar2=TWO_PI, op0=ALU.is_gt, op1=ALU.mult), [t_t], [t_t])
            V(lambda e: e.tensor_tensor(out=rr, in0=rr, in1=mm, op=ALU.subtract), [t_t], [t_t])
            V(lambda e: e.tensor_scalar(out=mm, in0=rr, scalar1=-math.pi, scalar2=TWO_PI, op0=ALU.is_lt, op1=ALU.mult), [t_t], [t_t])
            V(lambda e: e.tensor_tensor(out=rr, in0=rr, in1=mm, op=ALU.add), [t_t], [t_t])
            Sc(lambda e: e.activation(out=dst, in_=rr, func=ACTF.Sin), [t_t], [t_t, t_tab])
        sin_of(sinT, 0.0)
        sin_of(cosT, math.pi / 2)
        for a_ in (0, 2):
            V(lambda e, a_=a_: e.tensor_tensor(out=lv[:, a_, :], in0=lv[:, a_, :], in1=lv[:, a_ + 1, :], op=ALU.mult), [t_t], [t_t])
            V(lambda e, a_=a_: e.tensor_reduce(out=smalls[:, 32 + a_ // 2:33 + a_ // 2], in_=lv[:, a_, :], axis=AX.X, op=ALU.add), [t_t], [t_t])
        Sc(lambda e: e.activation(out=smalls[:, 34:36], in_=smalls[:, 32:34], func=ACTF.Exp), [t_t], [t_t])
        V(lambda e: e.tensor_tensor(out=smalls[:, 36:37], in0=smalls[:, 35:36], in1=smalls[:, 34:35], op=ALU.subtract), [t_t], [t_t])
        V(lambda e: e.tensor_scalar(out=lam_n, in0=smalls[:, 36:37], scalar1=-0.2, scalar2=None, op0=ALU.add), [t_t], [t_t, t_tab])
        Sc(lambda e: e.activation(out=esink, in_=smalls[:, 16:24], func=ACTF.Exp), [t_t], [t_t, t_tab])
        V(lambda e: e.tensor_scalar(out=subw, in0=smalls[:, 256:384], scalar1=0.8, scalar2=None, op0=ALU.mult), [t_t], [t_t, t_tab])
        P.barrier(exclude=[s_w])
        A.release(tm0)

        xt = [A.alloc([D]) for _ in range(2)]
        t_xt = [Trk(), Trk()]
        s_xt = [P.sem("s_xt0"), P.sem("s_xt1")]
        xs = A.alloc([D], BF16)
        junk = xs
        xsT = A.alloc([8, 128], BF16)
        proj = A.alloc([INW])
        tmpB = A.alloc([1664])
        roped2 = [A.alloc([1664], BF16) for _ in range(2)]
        QTd = A.alloc([4, 512], BF16)
        QTs = A.alloc([4, 512], BF16)
        NPT = 3
        PTp = [A.alloc([1024], BF16) for _ in range(NPT)]
        PT = [p_[:, 0:512] for p_ in PTp]
        t_PT = [Trk() for _ in range(NPT)]
        Oc2 = A.alloc([2, 4, 129])
        t_Oc2 = Trk()

        Oc = A.alloc([2, 4, 129])
        Os = A.alloc([8, 65])
        sm = A.alloc([64])
        od = tmpB[:, 0:512].rearrange("p (a b) -> p a b", b=128)
        od2 = tmpB[:, 512:1024].rearrange("p (a b) -> p a b", b=128)
        o_tok = A.alloc([4, D], BF16)
        oT = xsT
        h1t = proj[:, 0:D]
        s_h1 = P.sem("s_h1")
        t_xs, t_xsT, t_proj, t_tmpB, t_roped, t_QT, t_Oc, t_Os, t_sm, t_od, t_otok, t_oT, t_h1 = [Trk() for _ in range(13)]
        t_ss = Trk()
        Ocs = [Oc, Oc2]
        t_Ocs = [t_Oc, t_Oc2]
        t_projS, t_tmpBS, t_ropedS = Trk(), Trk(), Trk()
        t_roped2 = [Trk(), Trk()]
        t_ropedS2 = [Trk(), Trk()]
        pt_ctr = [0]

        t_wbf = Trk()
        s_wbf = P.sem("s_wbf")

        def stage_P1(I, js):
            i = 4 * I + js
            roped = roped2[js % 2]
            t_roped, t_ropedS = t_roped2[js % 2], t_ropedS2[js % 2]
            ntl = 4 * (NST if debug is None else debug.get("nst", NST))
            nzf = (PS + 128) // 128
            for zi in range(i * nzf // ntl, (i + 1) * nzf // ntl):
                P.dma("gpsimd", xs_d[zi * 128:(zi + 1) * 128, :].rearrange("p (a b) -> p a b", a=2), bc(zerob, 1, [128, 2, 512]), s_wbf, reads=[t_c])
            for ex_ in range(i * NE // ntl, (i + 1) * NE // ntl):
                P.dma("gpsimd", wbf[ex_ * 128:(ex_ + 1) * 128, 0:4096].rearrange("p (c n) -> p c n", c=8), w_gate[0, ex_].rearrange("(c p) n -> p c n", p=128), s_wbf)
                P.dma("gpsimd", wbf[ex_ * 128:(ex_ + 1) * 128, 4096:8192].rearrange("p (c n) -> p c n", c=8), w_up[0, ex_].rearrange("(c p) n -> p c n", p=128), s_wbf)
                P.dma("gpsimd", wbf[ex_ * 128:(ex_ + 1) * 128, 8192:12288].rearrange("p (c n) -> p c n", c=4), w_down[0, ex_].rearrange("(c p) n -> p c n", p=128), s_wbf)
            b = i % 2
            xtb = xt[b]
            P.dma("sync", xtb, x[i * 128:(i + 1) * 128, :], s_xt[b], writes=[t_xt[b]])
            ss = sm[:, 0:1]
            rstd = sm[:, 1:2]
            Sc(lambda e: e.activation(out=junk, in_=xtb, func=ACTF.Square, accum_out=ss), [t_xt[b]], [t_xs, t_ss])
            rstd_from_ss(ss, rstd, D, t_ss)
            V(lambda e: e.tensor_scalar(out=xs, in0=xtb, scalar1=rstd, scalar2=None, op0=ALU.mult), [t_xt[b], t_ss], [t_xs])
            tpv = bk_bf(0).rearrange("p (a b) -> p a b", b=128)
            for c in range(8):
                T(lambda e, c=c: e.transpose(tpv[:, c, :], xs[:, c * 128:(c + 1) * 128], identb), [t_xs, t_c], [tbank[0]], inc=(c == 7))
            Sc(lambda e: e.activation(out=xsT, in_=tpv, func=ACTF.Copy), [tbank[0]], [t_xsT])

        def stage_P2(I, js):
            i = 4 * I + js
            roped = roped2[js % 2]
            t_roped, t_ropedS = t_roped2[js % 2], t_ropedS2[js % 2]
            groups = [(0, 512), (512, 512), (1024, 512), (1536, 512), (2048, 256)]
            for gi, (c0, cw) in enumerate(groups):
                bkk = 2 + gi % 2
                for c in range(8):
                    T(lambda e, c=c, c0=c0, cw=cw, bkk=bkk: e.matmul(banks[bkk][:, 0:cw], lhsT=xsT[:, c, :], rhs=Win[:, c, c0:c0 + cw], start=(c == 0), stop=(c == 7)),
                      [t_xsT, t_w], [tbank[bkk]], inc=(c == 7))
                tpj = t_proj if gi < 3 else t_projS
                Sc(lambda e, c0=c0, cw=cw, bkk=bkk: e.activation(out=proj[:, c0:c0 + cw], in_=banks[bkk][:, 0:cw], func=ACTF.Copy), [tbank[bkk]], [tpj])
            Sc(lambda e: e.activation(out=Vda[:, i, :, 0:128], in_=proj[:, 1024:1536].rearrange("p (h d) -> p h d", h=4), func=ACTF.Copy), [t_proj], [t_kv])
            Sc(lambda e: e.activation(out=Vsa[:, 1 + js, :, 0:64], in_=proj[:, 2176:2304].rearrange("p (h d) -> p h d", h=2), func=ACTF.Copy), [t_projS], [t_kvs])

        def stage_P3(I, js):
            i = 4 * I + js
            roped = roped2[js % 2]
            t_roped, t_ropedS = t_roped2[js % 2], t_ropedS2[js % 2]
            cb = cosT[:, i, :]
            sb = sinT[:, i, :]
            for (opf, p0, t0, r0, U, t_proj_, t_tmpB_, t_roped_) in ((V, 0, 0, 0, 16, t_proj, t_tmpB, t_roped), (V, 1536, 1024, 1024, 10, t_projS, t_tmpBS, t_ropedS)):
                pv = proj[:, p0:p0 + U * 64].rearrange("p (u t f) -> p u t f", u=U, t=2, f=32)
                tv = tmpB[:, t0:t0 + U * 64].rearrange("p (u t f) -> p u t f", u=U, t=2, f=32)
                cb4 = cb.unsqueeze(1).unsqueeze(1).to_broadcast([128, U, 2, 32])
                sb4 = sb.unsqueeze(1).unsqueeze(1).to_broadcast([128, U, 2, 32])
                opf(lambda e, pv=pv, tv=tv, sb4=sb4: e.tensor_tensor(out=tv, in0=pv, in1=sb4, op=ALU.mult), [t_proj_, t_tab], [t_tmpB_])
                opf(lambda e, pv=pv, cb4=cb4: e.tensor_tensor(out=pv, in0=pv, in1=cb4, op=ALU.mult), [t_proj_, t_tab], [t_proj_])
                if U == 16:
                    parts = [(pv, tv, roped[:, 0:1024].rearrange("p (u t f) -> p u t f", u=16, t=2, f=32))]
                else:
                    pq = proj[:, 1536:2048].rearrange("p (k g t f) -> p k g t f", k=2, g=4, t=2, f=32)
                    tq = tmpB[:, 1024:1536].rearrange("p (k g t f) -> p k g t f", k=2, g=4, t=2, f=32)
                    rq = roped[:, 1024:1536].rearrange("p (g k t f) -> p k g t f", k=2, g=4, t=2, f=32)
                    pk = proj[:, 2048:2176].rearrange("p (u t f) -> p u t f", u=2, t=2, f=32)
                    tk = tmpB[:, 1536:1664].rearrange("p (u t f) -> p u t f", u=2, t=2, f=32)
                    rk = roped[:, 1536:1664].rearrange("p (u t f) -> p u t f", u=2, t=2, f=32)
                    parts = [(pq, tq, rq), (pk, tk, rk)]
                for (pp, tt, rv) in parts:
                    if len(pp.shape) == 5:
                        sl0 = (slice(None), slice(None), slice(None), 0, slice(None))
                        sl1 = (slice(None), slice(None), slice(None), 1, slice(None))
                    else:
                        sl0 = (slice(None), slice(None), 0, slice(None))
                        sl1 = (slice(None), slice(None), 1, slice(None))
                    opf(lambda e, pp=pp, tt=tt, rv=rv, sl0=sl0, sl1=sl1: e.tensor_tensor(out=rv[sl0], in0=pp[sl0], in1=tt[sl1], op=ALU.subtract), [t_proj_, t_tmpB_], [t_roped_])
                    opf(lambda e, pp=pp, tt=tt, rv=rv, sl0=sl0, sl1=sl1: e.tensor_tensor(out=rv[sl1], in0=pp[sl1], in1=tt[sl0], op=ALU.add), [t_proj_, t_tmpB_], [t_roped_])
            if i == 0:
                tap("xs", xs, t_xs)
                tap("xsT", xsT, t_xsT)
                tap("proj", proj, t_projS)
                tap("roped", roped, t_ropedS)
                tap("ropedD", roped[:, 0:1024], t_roped)
                tap("cos", cosT, t_tab)
                tap("sin", sinT, t_tab)
                tap("sm", sm, t_ss)

        def stage_P4(I, js):
            i = 4 * I + js
            roped = roped2[js % 2]
            t_roped, t_ropedS = t_roped2[js % 2], t_ropedS2[js % 2]
            tp0 = bk_bf(0).rearrange("p (a b) -> p a b", b=128)
            tp1 = bk_bf(1).rearrange("p (a b) -> p a b", b=128)
            for h in range(4):
                T(lambda e, h=h: e.transpose(tp0[:, h, :], roped[:, h * 128:(h + 1) * 128], identb), [t_roped, t_c], [tbank[0]], inc=False)
            for h in range(4):
                T(lambda e, h=h: e.transpose(tp0[:, 4 + h, :], roped[:, 512 + h * 128:512 + (h + 1) * 128], identb), [t_roped, t_c], [tbank[0]], inc=(h == 3))
            for g in range(4):
                T(lambda e, g=g: e.transpose(tp1[:, g, :], roped[:, 1024 + g * 128:1024 + (g + 1) * 128], identb), [t_ropedS, t_c], [tbank[1]], inc=False)
            T(lambda e: e.transpose(tp1[:, 4, :], roped[:, 1536:1664], identb), [t_ropedS, t_c], [tbank[1]])
            Sc(lambda e: e.activation(out=QTd[:, :, js * 128:(js + 1) * 128], in_=tp0[:, 0:4, :], func=ACTF.Copy), [tbank[0]], [t_QT])
            Sc(lambda e: e.activation(out=KTd[:, :, i * 128:(i + 1) * 128], in_=tp0[:, 4:8, :], func=ACTF.Copy), [tbank[0]], [t_kv])
            Sc(lambda e: e.activation(out=QTs[:, js, :], in_=bk_bf(1)[:, 0:512], func=ACTF.Copy), [tbank[1]], [t_QT])
            Sc(lambda e: e.activation(out=KTs[:, (1 + js) * 128:(2 + js) * 128], in_=tp1[:, 4, :], func=ACTF.Copy), [tbank[1]], [t_kvs])

        def stage_E(I):
            blocks = []
            L = 4 * I + 4
            for h in range(4):
                for kt in range(L):
                    for c in range(2):
                        blocks.append((h, c, kt, kt == 0, kt == L - 1))
            NB = len(blocks)

            def emit_S(n):
                h, c, kt, first, last = blocks[n]
                qlo = max(0, kt - 4 * I)
                bkk = n % 4
                diag = kt >= 4 * I
                T(lambda e: e.matmul(banks[bkk][:, qlo * 128:512], lhsT=KTd[c * 64:(c + 1) * 64, h, kt * 128:(kt + 1) * 128],
                                     rhs=QTd[c * 64:(c + 1) * 64, h, qlo * 128:512], start=True, stop=not diag),
                  [t_kv, t_QT], [tbank[bkk]], inc=not diag)
                if diag:
                    T(lambda e: e.matmul(banks[bkk][:, qlo * 128:(qlo + 1) * 128], lhsT=identb, rhs=cmask4[:, 0:128], start=False, stop=True),
                      [t_c], [tbank[bkk]])

            def emit_EXP(p):
                n = 2 * p
                h, c, kt, first, last = blocks[n]
                qlo = max(0, kt - 4 * I)
                k = pt_ctr[0] % NPT
                pt_ctr[0] += 1
                src = pp[p % 2].rearrange("p (c q) -> p c q", c=2)[:, :, qlo * 128:512]
                dst = PTp[k].rearrange("p (c q) -> p c q", c=2)[:, :, qlo * 128:512]
                Sc(lambda e: e.activation(out=dst, in_=src, func=ACTF.Exp, scale=0.125), [tbank[n % 4], tbank[(n + 1) % 4]], [t_PT[k]])
                return k

            def emit_PV(n, k):
                h, c, kt, first, last = blocks[n]
                qlo = max(0, kt - 4 * I)
                abs_ = (4 + 2 * c, 5 + 2 * c)
                if first:
                    for ab in abs_:
                        T(lambda e, ab=ab: e.matmul(banks[ab][:, 0:258], lhsT=zerob[0:1, 0:128], rhs=zerob[0:1, 0:258], start=True, stop=False, skip_group_check=True),
                          [t_c], [tbank[ab]], inc=False)
                for jq in range(qlo, 4):
                    ab = abs_[jq // 2]
                    av = banks[ab][:, 0:258].rearrange("p (a b) -> p a b", b=129)
                    T(lambda e, jq=jq, av=av: e.matmul(av[:, jq % 2, :], lhsT=PTp[k][:, c * 512 + jq * 128:c * 512 + (jq + 1) * 128], rhs=Vda[:, kt, h, :], start=False, stop=(kt == 4 * I + jq), skip_group_check=True),
                      [t_PT[k], t_kv], [tbank[ab]], inc=(jq == 3))
                if last:
                    for ai, ab in enumerate(abs_):
                        av = banks[ab][:, 0:258].rearrange("p (a b) -> p a b", b=129)
                        V(lambda e, ai=ai, av=av: e.tensor_copy(out=Ocs[h % 2][:, c, 2 * ai:2 * ai + 2, :], in_=av), [tbank[ab]], [t_Ocs[h % 2]])
                    if c == 1:
                        pend_fin.append((h, n // 2 + 5))

            def finalize_diff(h):
                rs = sm[:, 8:16].rearrange("p (c j) -> p c j", c=2)
                V(lambda e: e.reciprocal(out=rs, in_=Ocs[h % 2][:, :, :, 128]), [t_Ocs[h % 2]], [t_sm])
                V(lambda e: e.tensor_scalar(out=rs[:, 1, :], in0=rs[:, 1, :], scalar1=lam_n, scalar2=None, op0=ALU.mult), [t_sm, t_tab], [t_sm])
                V(lambda e: e.tensor_tensor(out=od, in0=Ocs[h % 2][:, 0, :, 0:128], in1=bc(rs[:, 0, :], 2, [128, 4, 128]), op=ALU.mult), [t_Ocs[h % 2], t_sm], [t_od])
                V(lambda e: e.tensor_tensor(out=od2, in0=Ocs[h % 2][:, 1, :, 0:128], in1=bc(rs[:, 1, :], 2, [128, 4, 128]), op=ALU.mult), [t_Ocs[h % 2], t_sm], [t_od])
                V(lambda e: e.tensor_tensor(out=od, in0=od, in1=od2, op=ALU.add), [t_od], [t_od])
                V(lambda e: e.tensor_tensor(out=od2, in0=od, in1=od, op=ALU.mult), [t_od], [t_od])
                ssd = sm[:, 16:20]
                V(lambda e: e.tensor_reduce(out=ssd, in_=od2, axis=AX.X, op=ALU.add), [t_od], [t_sm])
                rstd_from_ss(ssd, ssd, 128, t_sm)
                V(lambda e: e.tensor_tensor(out=od, in0=od, in1=bc(ssd, 2, [128, 4, 128]), op=ALU.mult), [t_od, t_sm], [t_od])
                V(lambda e: e.tensor_tensor(out=o_tok[:, :, h * 128:(h + 1) * 128], in0=od, in1=bc(subw, 1, [128, 4, 128]), op=ALU.mult), [t_od, t_tab], [t_otok])

            pend_fin = []
            for n in range(min(4, NB)):
                emit_S(n)
            NPr = NB // 2
            for p in range(NPr):
                k = emit_EXP(p)
                emit_PV(2 * p, k)
                emit_PV(2 * p + 1, k)
                if 2 * p + 4 < NB:
                    emit_S(2 * p + 4)
                    emit_S(2 * p + 5)
                while pend_fin and pend_fin[0][1] <= p:
                    finalize_diff(pend_fin.pop(0)[0])
            while pend_fin:
                finalize_diff(pend_fin.pop(0)[0])

        PTh = [PTp[k_][:, hh_ * 512:(hh_ + 1) * 512] for k_ in range(NPT) for hh_ in range(2)]
        t_PTh = [Trk() for _ in range(2 * NPT)]

        def swa_blocks(I, js):
            i = 4 * I + js
            out_ = []
            for kv in range(2):
                kts = ([(js, pmask4)] if i > 0 else []) + [(js + 1, cmask4)]
                for ti, (lb, mk) in enumerate(kts):
                    out_.append((kv, ti, lb, mk, len(kts)))
            return out_

        def swa_S(I, js):
            for bi, (kv, ti, lb, mk, nk) in enumerate(swa_blocks(I, js)):
                swa_S1(I, js, bi, kv, lb, mk)

        def swa_S1(I, js, bi, kv, lb, mk):
            sv = banks[bi][:, 0:512]
            T(lambda e: e.matmul(sv, lhsT=KTs[kv * 64:(kv + 1) * 64, lb * 128:(lb + 1) * 128],
                                 rhs=QTs[kv * 64:(kv + 1) * 64, js, :], start=True, stop=False),
              [t_kvs, t_QT], [tbank[bi]], inc=False)
            T(lambda e: e.matmul(sv, lhsT=identb, rhs=mk, start=False, stop=True), [t_c], [tbank[bi]])

        def swa_EXP(I, js):
            for bi, (kv, ti, lb, mk, nk) in enumerate(swa_blocks(I, js)):
                Sc(lambda e, bi=bi: e.activation(out=PTh[bi], in_=banks[bi][:, 0:512], func=ACTF.Exp, scale=0.125), [tbank[bi]], [t_PTh[bi], t_PT[bi // 2]])

        def swa_PV(I, js):
            for kv in range(2):
                ab = 6 + kv
                T(lambda e, ab=ab: e.matmul(banks[ab][:, 0:260], lhsT=zerob[0:1, 0:128], rhs=zerob[0:1, 0:260], start=True, stop=False, skip_group_check=True),
                  [t_c], [tbank[ab]], inc=False)
            for bi, (kv, ti, lb, mk, nk) in enumerate(swa_blocks(I, js)):
                swa_PV1(I, js, bi, kv, ti, lb, nk)

        def swa_PV1(I, js, bi, kv, ti, lb, nk):
            ab = 6 + kv
            av = banks[ab][:, 0:260].rearrange("p (a b) -> p a b", b=65)
            for g in range(4):
                T(lambda e, g=g: e.matmul(av[:, g, :], lhsT=PTh[bi][:, g * 128:(g + 1) * 128], rhs=Vsa[:, lb, kv, :], start=False,
                                          stop=(ti == nk - 1), skip_group_check=True),
                  [t_PTh[bi], t_PT[bi // 2], t_kvs], [tbank[ab]], inc=(g == 3))

        def swa_FIN(I, js):
            for kv in range(2):
                ab = 6 + kv
                av = banks[ab][:, 0:260].rearrange("p (a b) -> p a b", b=65)
                V(lambda e, av=av, kv=kv: e.tensor_copy(out=Os[:, kv * 4:(kv + 1) * 4, :], in_=av), [tbank[ab]], [t_Os])
            den = sm[:, 24:32]
            V(lambda e: e.tensor_tensor(out=den, in0=Os[:, :, 64], in1=esink, op=ALU.add), [t_Os, t_tab], [t_sm])
            V(lambda e: e.reciprocal(out=den, in_=den), [t_sm], [t_sm])
            V(lambda e: e.tensor_tensor(out=o_tok[:, js, 512:1024].rearrange("p (h d) -> p h d", h=8), in0=Os[:, :, 0:64], in1=bc(den, 2, [128, 8, 64]), op=ALU.mult),
              [t_Os, t_sm], [t_otok])

        def stage_F(I):
            swa_S(I, 0)
            swa_EXP(I, 0)
            for js in range(4):
                swa_PV(I, js)
                if js + 1 < 4:
                    swa_S(I, js + 1)
                swa_FIN(I, js)
                if js + 1 < 4:
                    swa_EXP(I, js + 1)
            V(lambda e: e.tensor_copy(out=KTs[:, 0:128], in_=KTs[:, 512:640]), [t_kvs], [t_kvs])
            V(lambda e: e.tensor_copy(out=Vsa[:, 0, :, :], in_=Vsa[:, 4, :, :]), [t_kvs], [t_kvs])

        oT2 = [xsT, xs.rearrange("p (a b) -> p a b", b=128)]
        t_oT2 = [t_xsT, t_xs]

        def H1(I, js):
            i = 4 * I + js
            x2t = xt[js % 2]
            t_x2 = t_xt[js % 2]
            P.dma("sync", x2t, x[i * 128:(i + 1) * 128, :], s_xt[js % 2], writes=[t_x2])
            tpv = bk_bf(1).rearrange("p (a b) -> p a b", b=128)
            for c in range(8):
                T(lambda e, c=c: e.transpose(tpv[:, c, :], o_tok[:, js, c * 128:(c + 1) * 128], identb), [t_otok, t_c], [tbank[1]], inc=(c == 7))
            V(lambda e: e.tensor_copy(out=oT2[js % 2], in_=tpv), [tbank[1]], [t_oT2[js % 2]])

        def H2(I, js):
            i = 4 * I + js
            x2t = xt[js % 2]
            t_x2 = t_xt[js % 2]
            oT_, t_oT_ = oT2[js % 2], t_oT2[js % 2]
            for hf in range(2):
                bkk = 2 + hf
                for c in range(8):
                    T(lambda e, c=c, hf=hf, bkk=bkk: e.matmul(banks[bkk][:, 0:512], lhsT=oT_[:, c, :], rhs=Wout[:, c, hf * 512:(hf + 1) * 512], start=(c == 0), stop=(c == 7)),
                      [t_oT_, t_w], [tbank[bkk]], inc=(c == 7))
                V(lambda e, hf=hf, bkk=bkk: e.tensor_tensor(out=h1t[:, hf * 512:(hf + 1) * 512], in0=banks[bkk][:, 0:512], in1=x2t[:, hf * 512:(hf + 1) * 512], op=ALU.add),
                  [tbank[bkk], t_x2], [t_h1, t_proj])
            P.dma("scalar", hbuf[i * 128:(i + 1) * 128, :], h1t, s_h1, reads=[t_h1, t_proj])

        def stage_H(I):
            H1(I, 0)
            for js in range(4):
                if js + 1 < 4:
                    H1(I, js + 1)
                H2(I, js)

        _top = A.top
        A.release(m_persist)
        Wq_e = A.alloc([8, D], BF16)
        Wo_e = A.alloc([8, D], BF16)
        A.top = _top
        t_w2 = Trk()
        s_w2 = P.sem("s_w2")
        n_st = NST if debug is None else debug.get("nst", NST)
        for I in range(n_st):
            stage_P1(I, 0)
            stage_P2(I, 0)
            for js in range(4):
                if js + 1 < 4:
                    stage_P1(I, js + 1)
                stage_P3(I, js)
                if js + 1 < 4:
                    stage_P2(I, js + 1)
                stage_P4(I, js)
            if I == 0:
                tap("QTd", QTd, t_QT)
                tap("QTs", QTs, t_QT)
                tap("KTd", KTd[:, :, 0:512], t_kv)
                tap("Vda", Vda[:, 0:4, :, :], t_kv)
            if I == n_st - 1:
                P.dma("gpsimd", Wq_e, wq_cross[0].rearrange("(c p) n -> p c n", p=128), s_w2, writes=[t_w2, t_w])
                P.dma("gpsimd", Wo_e, wo_cross[0].rearrange("(c p) n -> p c n", p=128), s_w2, writes=[t_w2, t_w])
            stage_E(I)
            stage_F(I)
            if I == 0:
                tap("otok", o_tok, t_otok)
                tap("Oc", Oc, t_Oc)
            stage_H(I)

        if debug is not None and debug.get("stop") == "1A":
            P.barrier()
            finish_copy(P, A, out, hbuf, n_st * 4)
            P.build()
            return
        P.barrier()
        A.release(m_persist)

        Wq = A.alloc([8, D], BF16)
        Wo = A.alloc([8, D], BF16)
        KcT = A.alloc([8, 256], BF16)
        Vca = A.alloc([2, 4, 257], BF16)
        lncross_b = A.alloc([D])
        lnmoe_b = A.alloc([D])
        bias_b = A.alloc([36])
        Wr = A.alloc([8, 36])
        t_kc, t_tab2 = Trk(), Trk()
        s_t2 = P.sem("s_t2")
        assert str(Wq) == str(Wq_e) and str(Wo) == str(Wo_e)
        P.dma("sync", lncross_b, ln_cross_w.partition_broadcast(128), s_t2, writes=[t_tab2])
        P.dma("sync", lnmoe_b, ln_moe_w.partition_broadcast(128), s_t2, writes=[t_tab2])
        P.dma("sync", bias_b[:, 0:4], b_group.partition_broadcast(128), s_t2, writes=[t_tab2])
        P.dma("sync", bias_b[:, 4:36], b_expert.partition_broadcast(128), s_t2, writes=[t_tab2])
        P.dma("sync", Wr[:, :, 0:4], w_group[0].rearrange("(c p) n -> p c n", p=128), s_t2, writes=[t_tab2])
        P.dma("sync", Wr[:, :, 4:36], w_expert[0].rearrange("(c p) n -> p c n", p=128), s_t2, writes=[t_tab2])
        G(lambda e: e.memset(Vca, 1.0), [], [t_kc])
        sm2 = A.alloc([64])
        t_sm2 = Trk()
        m1b = A.mark()
        Wkv = A.alloc([8, 2 * D], BF16)
        memt = A.alloc([2, D])
        lnmem_b = A.alloc([D])
        msb = A.alloc([D], BF16)
        msT = A.alloc([8, 256], BF16)
        t_m, t_ms, t_msT, t_mw = Trk(), Trk(), Trk(), Trk()
        s_m = P.sem("s_m")
        s_m2 = P.sem("s_m2")
        P.dma("gpsimd", Wkv, wkv_cross[0].rearrange("(c p) n -> p c n", p=128), s_m2, writes=[t_mw])
        P.dma("sync", memt, mem.rearrange("(t p) d -> p t d", p=128), s_m, writes=[t_m])
        P.dma("sync", lnmem_b, ln_mem_w.partition_broadcast(128), s_m, writes=[t_m])

        def mem_tile(mt):
            ss = sm2[:, 0:1]
            rstd = sm2[:, 1:2]
            Sc(lambda e: e.activation(out=msb, in_=memt[:, mt, :], func=ACTF.Square, accum_out=ss), [t_m], [t_ms, t_sm2])
            rstd_from_ss(ss, rstd, D, t_sm2)
            V(lambda e: e.scalar_tensor_tensor(out=msb, in0=memt[:, mt, :], scalar=rstd, in1=lnmem_b, op0=ALU.mult, op1=ALU.mult), [t_m, t_sm2], [t_ms])
            tpv = bk_bf(0).rearrange("p (a b) -> p a b", b=128)
            for c in range(8):
                T(lambda e, c=c: e.transpose(tpv[:, c, :], msb[:, c * 128:(c + 1) * 128], identb), [t_ms, t_c], [tbank[0]], inc=(c == 7))
            V(lambda e: e.tensor_copy(out=msT[:, :, mt * 128:(mt + 1) * 128], in_=tpv), [tbank[0]], [t_msT])
        for mt in range(2):
            mem_tile(mt)

        def kc_chunk(fc):
            bkk = 2 + fc % 2
            for c in range(8):
                T(lambda e, c=c: e.matmul(banks[bkk][:, 0:256], lhsT=Wkv[:, c, fc * 128:(fc + 1) * 128], rhs=msT[:, c, :], start=(c == 0), stop=(c == 7)),
                  [t_mw, t_msT], [tbank[bkk]], inc=(c == 7))
            V(lambda e: e.tensor_copy(out=KcT[:, fc, :], in_=banks[bkk][:, 0:256]), [tbank[bkk]], [t_kc])
        for fc in range(8):
            kc_chunk(fc)

        def vc_chunk(mt, hf):
            bkk = 2 + hf
            for c in range(8):
                T(lambda e, c=c: e.matmul(banks[bkk][:, 0:512], lhsT=msT[:, c, mt * 128:(mt + 1) * 128], rhs=Wkv[:, c, D + hf * 512:D + (hf + 1) * 512], start=(c == 0), stop=(c == 7)),
                  [t_mw, t_msT], [tbank[bkk]], inc=(c == 7))
            Sc(lambda e: e.activation(out=Vca[:, mt, 2 * hf:2 * hf + 2, 0:256], in_=banks[bkk][:, 0:512].rearrange("p (a b) -> p a b", b=256), func=ACTF.Copy), [tbank[bkk]], [t_kc])
        for mt in range(2):
            for hf in range(2):
                vc_chunk(mt, hf)
        P.barrier()
        A.release(m1b)

        F_all = A.alloc([NT, D], BF16)
        Lg_all = A.alloc([NT, 36])
        eidf = A.alloc([NT * 2])
        posv = A.alloc([NT * 2])
        cum = A.alloc([32])
        m_loop = A.mark()
        ht2 = [A.alloc([4, D]) for _ in range(2)]
        t_ht2 = [[Trk() for _ in range(4)] for _ in range(2)]
        s_ht2 = [[P.sem(f"s_ht{q}{j}") for j in range(4)] for q in range(2)]
        s_h22 = [[P.sem(f"s_h2{q}{j}") for j in range(4)] for q in range(2)]
        hs = A.alloc([D], BF16)
        hs_r = A.alloc([D], BF16)
        hsT_ = A.alloc([8, 512], BF16)
        hsT2 = [hsT_, hsT_]
        t_hsT_ = Trk()
        t_hsT2 = [t_hsT_, t_hsT_]
        t_hsr, t_smL, t_smR, t_smP = Trk(), Trk(), Trk(), Trk()
        Ohc = A.alloc([4, 257])
        t_Ohc = Trk()
        qcT = A.alloc([8, 512], BF16)
        PcT = [A.alloc([512], BF16) for _ in range(4)]
        t_PcT = [Trk() for _ in range(4)]
        oc_tok = A.alloc([4, D], BF16)
        ocT = A.alloc([8, 128], BF16)
        hm = A.alloc([D])
        hmT = A.alloc([8, 128])
        t_hs, t_hsT, t_qcT, t_octok, t_ocT, t_hm, t_hmT, t_L, t_rt, t_F, t_cum, t_Mb = [Trk() for _ in range(12)]
        V(lambda e: e.tensor_copy(out=Lg_all[:, :, 0:4], in_=bc(cstt[:, C_IO:C_IO + 4], 1, [128, NT, 4])), [t_c], [t_L])
        V(lambda e: e.tensor_copy(out=Lg_all[:, :, 4:36], in_=bc(cstt[:, C_IO:C_IO + 32], 1, [128, NT, 32])), [t_c], [t_L])
        pc_ctr = [0]
        iota_e = cstt[:, C_IO:C_IO + 32]

        def b_load_a(I, js):
            i = 4 * I + js
            ht, t_ht, s_ht = ht2[I % 2], t_ht2[I % 2], s_ht2[I % 2]
            P.dma("sync", ht[:, js, :], hbuf[i * 128:(i + 1) * 128, :], s_ht[js], writes=[t_ht[js]])
            ss = sm2[:, 0:1]
            rstd = sm2[:, 1:2]
            Sc(lambda e: e.activation(out=hs, in_=ht[:, js, :], func=ACTF.Square, accum_out=ss), [t_ht[js]], [t_hs, t_smL])
            rstd_from_ss(ss, rstd, D, t_smL)
            V(lambda e: e.scalar_tensor_tensor(out=hs, in0=ht[:, js, :], scalar=rstd, in1=lncross_b, op0=ALU.mult, op1=ALU.mult), [t_ht[js], t_smL, t_tab2], [t_hs])

        def b_load_b(I, js):
            hsT, t_hsT = hsT2[I % 2], t_hsT2[I % 2]
            tpv = bk_bf(0).rearrange("p (a b) -> p a b", b=128)
            for c in range(8):
                T(lambda e, c=c: e.transpose(tpv[:, c, :], hs[:, c * 128:(c + 1) * 128], identb), [t_hs, t_c], [tbank[0]], inc=(c == 7))
            V(lambda e: e.tensor_copy(out=hsT[:, :, js * 128:(js + 1) * 128], in_=tpv), [tbank[0]], [t_hsT])

        def b_qproj(I, fc, bb=2):
            hsT, t_hsT = hsT2[I % 2], t_hsT2[I % 2]
            bkk = bb + fc % 2
            for c in range(8):
                T(lambda e, c=c: e.matmul(banks[bkk][:, 0:512], lhsT=Wq[:, c, fc * 128:(fc + 1) * 128], rhs=hsT[:, c, :], start=(c == 0), stop=(c == 7)),
                  [t_w2, t_hsT], [tbank[bkk]], inc=(c == 7))
            if fc % 2 == 0:
                Sc(lambda e: e.activation(out=qcT[:, fc, :], in_=banks[bkk][:, 0:512], func=ACTF.Copy), [tbank[bkk]], [t_qcT])
            else:
                V(lambda e: e.tensor_copy(out=qcT[:, fc, :], in_=banks[bkk][:, 0:512]), [tbank[bkk]], [t_qcT])

        def b_score(hc, mt):
            bkk = 4 + mt
            for u in range(2):
                T(lambda e, u=u: e.matmul(banks[bkk][:, 0:512], lhsT=KcT[:, 2 * hc + u, mt * 128:(mt + 1) * 128], rhs=qcT[:, 2 * hc + u, :], start=(u == 0), stop=(u == 1)),
                  [t_kc, t_qcT], [tbank[bkk]], inc=(u == 1))
            k = pc_ctr[0] % 4
            pc_ctr[0] += 1
            Sc(lambda e: e.activation(out=PcT[k], in_=banks[bkk][:, 0:512], func=ACTF.Exp, scale=1.0 / 16.0), [tbank[bkk]], [t_PcT[k]])
            return k

        def b_pv(hc, js, ks):
            ab = 6 + js % 2
            for mt in range(2):
                T(lambda e, mt=mt: e.matmul(banks[ab][:, 0:257], lhsT=PcT[ks[mt]][:, js * 128:(js + 1) * 128], rhs=Vca[:, mt, hc, :], start=(mt == 0), stop=(mt == 1)),
                  [t_PcT[ks[mt]], t_kc], [tbank[ab]], inc=(mt == 1))
            Sc(lambda e: e.activation(out=Ohc[:, js, :], in_=banks[ab][:, 0:257], func=ACTF.Copy), [tbank[ab]], [t_Ohc])
            if js == 3:
                rd = sm2[:, 4:8]
                V(lambda e: e.reciprocal(out=rd, in_=Ohc[:, :, 256]), [t_Ohc], [t_smP])
                V(lambda e: e.tensor_tensor(out=oc_tok[:, :, hc * 256:(hc + 1) * 256], in0=Ohc[:, :, 0:256], in1=bc(rd, 2, [128, 4, 256]), op=ALU.mult), [t_Ohc, t_smP], [t_octok])

        def b_O1(I, js):
            tpv = bk_bf(1).rearrange("p (a b) -> p a b", b=128)
            for c in range(8):
                T(lambda e, c=c: e.transpose(tpv[:, c, :], oc_tok[:, js, c * 128:(c + 1) * 128], identb), [t_octok, t_c], [tbank[1]], inc=(c == 7))
            V(lambda e: e.tensor_copy(out=ocT, in_=tpv), [tbank[1]], [t_ocT])

        def b_O2(I, js):
            i = 4 * I + js
            ht, t_ht, s_h2 = ht2[I % 2], t_ht2[I % 2], s_h22[I % 2]
            for hf in range(2):
                bkk = 2 + hf
                for c in range(8):
                    T(lambda e, c=c, hf=hf, bkk=bkk: e.matmul(banks[bkk][:, 0:512], lhsT=ocT[:, c, :], rhs=Wo[:, c, hf * 512:(hf + 1) * 512], start=(c == 0), stop=(c == 7)),
                      [t_ocT, t_w2], [tbank[bkk]], inc=(c == 7))
                V(lambda e, hf=hf, bkk=bkk: e.tensor_tensor(out=ht[:, js, hf * 512:(hf + 1) * 512], in0=banks[bkk][:, 0:512], in1=ht[:, js, hf * 512:(hf + 1) * 512], op=ALU.add),
                  [tbank[bkk], t_ht[js]], [t_ht[js]])
            P.dma("scalar", hbuf[i * 128:(i + 1) * 128, :], ht[:, js, :], s_h2[js], reads=[t_ht[js]])

        def b_R1(I, js):
            i = 4 * I + js
            ht, t_ht = ht2[I % 2], t_ht2[I % 2]
            h2 = ht[:, js, :]
            ss = sm2[:, 2:3]
            rstd = sm2[:, 3:4]
            V(lambda e: e.tensor_tensor(out=hm, in0=h2, in1=lnmoe_b, op=ALU.mult), [t_ht[js], t_tab2], [t_hm])
            Sc(lambda e: e.activation(out=hs_r, in_=h2, func=ACTF.Square, accum_out=ss), [t_ht[js]], [t_hsr, t_smR])
            rstd_from_ss(ss, rstd, D, t_smR)
            Sc(lambda e: e.activation(out=F_all[:, i, :], in_=hm, func=ACTF.Copy, scale=rstd), [t_hm, t_smR], [t_F])
            for half in range(2):
                bkk = 6 + half
                for c4 in range(4):
                    c = half * 4 + c4
                    T(lambda e, c=c, c4=c4, bkk=bkk: e.transpose(banks[bkk][:, c4 * 128:(c4 + 1) * 128], hm[:, c * 128:(c + 1) * 128], identf), [t_hm, t_c], [tbank[bkk]], inc=(c4 == 3))
                if half == 0:
                    V(lambda e: e.tensor_copy(out=hmT[:, 0:4, :], in_=banks[6][:, 0:512].rearrange("p (a b) -> p a b", b=128)), [tbank[6]], [t_hmT])
                else:
                    Sc(lambda e: e.activation(out=hmT[:, 4:8, :], in_=banks[7][:, 0:512].rearrange("p (a b) -> p a b", b=128), func=ACTF.Copy), [tbank[7]], [t_hmT])

        def b_R2(I, js):
            i = 4 * I + js
            rstd = sm2[:, 3:4]
            for c in range(8):
                T(lambda e, c=c: e.matmul(banks[4][:, 0:36], lhsT=hmT[:, c, :], rhs=Wr[:, c, :], start=(c == 0), stop=(c == 7)), [t_hmT, t_tab2], [tbank[4]], inc=(c == 7))
            V(lambda e: e.scalar_tensor_tensor(out=Lg_all[:, i, :], in0=banks[4][:, 0:36], scalar=rstd, in1=bias_b, op0=ALU.mult, op1=ALU.add), [tbank[4], t_smR, t_tab2], [t_L])

        n_st = NST if debug is None else debug.get("nst", NST)
        for js in range(4):
            b_load_a(0, js)
            b_load_b(0, js)
        for fc in range(8):
            b_qproj(0, fc)
        for I in range(n_st):
            nxt = I + 1 < n_st
            for hc in range(4):
                ks = [b_score(hc, 0), b_score(hc, 1)]
                if nxt:
                    b_load_a(I + 1, hc)
                for js in range(4):
                    b_pv(hc, js, ks)
                if nxt:
                    b_load_b(I + 1, hc)
            b_O1(I, 0)
            for js in range(4):
                b_O2(I, js)
                if js + 1 < 4:
                    b_O1(I, js + 1)
                if nxt:
                    b_qproj(I + 1, 2 * js, 4)
                b_R1(I, js)
                if nxt:
                    b_qproj(I + 1, 2 * js + 1, 4)
                b_R2(I, js)
            if I == 0:
                tap("octok", oc_tok, t_octok)

        if debug is not None and debug.get("stop") == "1B":
            P.barrier()
            finish_copy(P, A, out, hbuf, n_st * 4)
            P.build()
            return

        P.barrier()
        A.release(m_loop)
        N_ = NT
        rA = lambda n_: A.alloc([n_])
        gmax, gsum, m1, m2, dl, ex, rden, g1 = [rA(N_) for _ in range(8)]
        gm = A.alloc([N_, 4])
        gex = A.alloc([N_, 4])
        tmp48 = A.alloc([N_, 4, 8])
        esel, mk1, e2, mk2 = [A.alloc([N_, 8]) for _ in range(4)]
        M1 = A.alloc([N_, 4, 8])
        M2 = A.alloc([N_, 4, 8])
        Msum = A.alloc([N_, 32])
        Mb_all = A.alloc([N_ * 32], BF16)
        pit = A.alloc([N_, 32])
        totA = A.alloc([N_, 32])
        totB = A.alloc([N_, 32])
        tot0 = A.alloc([N_, 32])
        t_r = Trk()
        rw = [t_r]
        gl = Lg_all[:, :, 0:4]
        el = Lg_all[:, :, 4:36].rearrange("p i (g e) -> p i g e", g=4)
        V(lambda e: e.tensor_reduce(out=gmax, in_=gl, axis=AX.X, op=ALU.max), [t_L], rw)
        V(lambda e: e.tensor_tensor(out=gm, in0=gl, in1=bc(gmax, 2, [128, N_, 4]), op=ALU.is_equal), [t_L] + rw, rw)
        V(lambda e: e.tensor_tensor(out=gex, in0=gl, in1=bc(gmax, 2, [128, N_, 4]), op=ALU.subtract), [t_L] + rw, rw)
        Sc(lambda e: e.activation(out=gex, in_=gex, func=ACTF.Exp), rw, rw)
        V(lambda e: e.tensor_reduce(out=gsum, in_=gex, axis=AX.X, op=ALU.add), rw, rw)
        V(lambda e: e.tensor_tensor(out=tmp48, in0=el, in1=bc(gm, 3, [128, N_, 4, 8]), op=ALU.mult), [t_L] + rw, rw)
        V(lambda e: e.tensor_reduce(out=esel, in_=tmp48.rearrange("p i g e -> p i e g"), axis=AX.X, op=ALU.add), rw, rw)
        V(lambda e: e.tensor_reduce(out=m1, in_=esel, axis=AX.X, op=ALU.max), rw, rw)
        V(lambda e: e.tensor_tensor(out=mk1, in0=esel, in1=bc(m1, 2, [128, N_, 8]), op=ALU.is_equal), rw, rw)
        V(lambda e: e.scalar_tensor_tensor(out=e2, in0=mk1, scalar=-1e30, in1=esel, op0=ALU.mult, op1=ALU.add), rw, rw)
        V(lambda e: e.tensor_reduce(out=m2, in_=e2, axis=AX.X, op=ALU.max), rw, rw)
        V(lambda e: e.tensor_tensor(out=mk2, in0=e2, in1=bc(m2, 2, [128, N_, 8]), op=ALU.is_equal), rw, rw)
        V(lambda e: e.tensor_tensor(out=dl, in0=m2, in1=m1, op=ALU.subtract), rw, rw)
        Sc(lambda e: e.activation(out=ex, in_=dl, func=ACTF.Exp), rw, rw)
        V(lambda e: e.tensor_scalar(out=rden, in0=ex, scalar1=1.0, scalar2=None, op0=ALU.add), rw, rw)
        V(lambda e: e.tensor_tensor(out=rden, in0=rden, in1=gsum, op=ALU.mult), rw, rw)
        V(lambda e: e.reciprocal(out=g1, in_=rden), rw, rw)
        V(lambda e: e.tensor_copy(out=gates[:, :, 0], in_=g1), rw, [t_gd])
        V(lambda e: e.tensor_tensor(out=gates[:, :, 1], in0=g1, in1=ex, op=ALU.mult), rw, [t_gd])
        V(lambda e: e.tensor_tensor(out=M1, in0=bc(gm, 3, [128, N_, 4, 8]), in1=bc(mk1, 2, [128, N_, 4, 8]), op=ALU.mult), rw, rw)
        V(lambda e: e.tensor_tensor(out=M2, in0=bc(gm, 3, [128, N_, 4, 8]), in1=bc(mk2, 2, [128, N_, 4, 8]), op=ALU.mult), rw, rw)
        M1f = M1.rearrange("p i g e -> p i (g e)")
        M2f = M2.rearrange("p i g e -> p i (g e)")
        V(lambda e: e.tensor_tensor(out=Msum, in0=M1f, in1=M2f, op=ALU.add), rw, rw)
        V(lambda e: e.tensor_copy(out=Mb_all, in_=Msum.rearrange("p i e -> p (i e)")), rw, rw)
        for hf in range(2):
            T(lambda e, hf=hf: e.matmul(banks[hf][:, 0:512], lhsT=ltrib, rhs=Mb_all[:, hf * 512:(hf + 1) * 512], start=True, stop=True), rw + [t_c], [tbank[hf]])
            T(lambda e, hf=hf: e.matmul(banks[2 + hf][:, 0:512], lhsT=onesb, rhs=Mb_all[:, hf * 512:(hf + 1) * 512], start=True, stop=True), rw + [t_c], [tbank[2 + hf]])
            V(lambda e, hf=hf: e.tensor_copy(out=pit[:, hf * 16:(hf + 1) * 16, :], in_=banks[hf][:, 0:512].rearrange("p (i e) -> p i e", e=32)), [tbank[hf]], rw)
            V(lambda e, hf=hf: e.tensor_copy(out=tot0[:, hf * 16:(hf + 1) * 16, :], in_=banks[2 + hf][:, 0:512].rearrange("p (i e) -> p i e", e=32)), [tbank[2 + hf]], rw)
        V(lambda e: e.tensor_copy(out=totA, in_=tot0), rw, rw)
        sA, sB = totA, totB
        for sft in (1, 2, 4, 8, 16):
            V(lambda e, sA=sA, sB=sB, sft=sft: e.tensor_copy(out=sB[:, 0:sft, :], in_=sA[:, 0:sft, :]), rw, rw)
            V(lambda e, sA=sA, sB=sB, sft=sft: e.tensor_tensor(out=sB[:, sft:N_, :], in0=sA[:, sft:N_, :], in1=sA[:, 0:N_ - sft, :], op=ALU.add), rw, rw)
            sA, sB = sB, sA
        incl = sA
        V(lambda e: e.tensor_copy(out=cum, in_=incl[:, N_ - 1, :]), rw, [t_cum])
        V(lambda e: e.tensor_tensor(out=tot0, in0=incl, in1=tot0, op=ALU.subtract), rw, rw)
        V(lambda e: e.tensor_tensor(out=pit, in0=pit, in1=tot0, op=ALU.add), rw, rw)
        posv3 = posv.rearrange("p (i k) -> p i k", k=2)
        eidf3 = eidf.rearrange("p (i k) -> p i k", k=2)
        for k_, Mk in enumerate((M1f, M2f)):
            V(lambda e, Mk=Mk: e.tensor_tensor(out=Msum, in0=Mk, in1=pit, op=ALU.mult), rw, rw)
            V(lambda e, k_=k_: e.tensor_reduce(out=posv3[:, :, k_], in_=Msum, axis=AX.X, op=ALU.add), rw, [t_gd])
            V(lambda e, Mk=Mk: e.tensor_tensor(out=Msum, in0=Mk, in1=bc(iota_e, 1, [128, N_, 32]), op=ALU.mult), rw + [t_c], rw)
            V(lambda e, k_=k_: e.tensor_reduce(out=eidf3[:, :, k_], in_=Msum, axis=AX.X, op=ALU.add), rw, [t_gd])

        fz = A.alloc([2048])
        cA, cB = fz[:, 0:32], fz[:, 32:64]
        qf, qi_, corr = fz[:, 64:96], fz[:, 96:128].bitcast(I32), fz[:, 128:160]
        padded, pst = fz[:, 160:192], fz[:, 192:224]
        destf = fz[:, 256:320]
        bef = fz[:, 320:320 + NBLK]
        chf = fz[:, 416:416 + NBLK]
        big = A.alloc([NBLK, 32])
        t_fz = Trk()
        rwf = [t_fz]
        V(lambda e: e.tensor_scalar(out=cA, in0=cum, scalar1=float(US - 1), scalar2=1.0 / US, op0=ALU.add, op1=ALU.mult), [t_cum], rwf)
        V(lambda e: e.tensor_copy(out=qi_, in_=cA), rwf, rwf)
        V(lambda e: e.tensor_copy(out=qf, in_=qi_), rwf, rwf)
        V(lambda e: e.tensor_tensor(out=corr, in0=qf, in1=cA, op=ALU.is_gt), rwf, rwf)
        V(lambda e: e.tensor_tensor(out=qf, in0=qf, in1=corr, op=ALU.subtract), rwf, rwf)
        V(lambda e: e.tensor_scalar(out=padded, in0=qf, scalar1=float(US), scalar2=None, op0=ALU.mult), rwf, rwf)
        V(lambda e: e.tensor_copy(out=cA, in_=padded), rwf, rwf)
        src_, dst_ = cA, cB
        for sft in (1, 2, 4, 8, 16):
            V(lambda e, src_=src_, dst_=dst_, sft=sft: e.tensor_copy(out=dst_[:, 0:sft], in_=src_[:, 0:sft]), rwf, rwf)
            V(lambda e, src_=src_, dst_=dst_, sft=sft: e.tensor_tensor(out=dst_[:, sft:32], in0=src_[:, sft:32], in1=src_[:, 0:32 - sft], op=ALU.add), rwf, rwf)
            src_, dst_ = dst_, src_
        pends = src_
        V(lambda e: e.tensor_tensor(out=pst, in0=pends, in1=padded, op=ALU.subtract), rwf, rwf)
        b64 = big[:, 0:64, :]
        V(lambda e: e.tensor_tensor(out=b64, in0=bc(iota_e, 1, [128, 64, 32]), in1=bc(eidf, 2, [128, 64, 32]), op=ALU.is_equal), [t_gd, t_c], rwf)
        V(lambda e: e.tensor_tensor(out=b64, in0=b64, in1=bc(pst, 1, [128, 64, 32]), op=ALU.mult), rwf, rwf)
        V(lambda e: e.tensor_reduce(out=destf, in_=b64, axis=AX.X, op=ALU.add), rwf, rwf)
        V(lambda e: e.tensor_tensor(out=destf, in0=destf, in1=posv, op=ALU.add), rwf + [t_gd], rwf)
        V(lambda e: e.tensor_copy(out=dest, in_=destf), rwf, [t_gd])
        V(lambda e: e.tensor_tensor(out=big, in0=bc(pends, 1, [128, NBLK, 32]), in1=bc(cstt[:, C_BS:C_BS + NBLK], 2, [128, NBLK, 32]), op=ALU.is_le), rwf + [t_c], rwf)
        V(lambda e: e.tensor_reduce(out=bef, in_=big, axis=AX.X, op=ALU.add), rwf, rwf)
        V(lambda e: e.tensor_scalar(out=bef, in0=bef, scalar1=31.0, scalar2=None, op0=ALU.min), rwf, rwf)
        V(lambda e: e.memset(chf, 1.0), [], rwf)
        V(lambda e: e.tensor_tensor(out=chf[:, 2:NBLK], in0=bef[:, 2:NBLK], in1=bef[:, 0:NBLK - 2], op=ALU.not_equal), rwf, rwf)
        V(lambda e: e.tensor_copy(out=blkE, in_=bef), rwf, [t_gd])
        V(lambda e: e.tensor_scalar(out=bef, in0=bef, scalar1=128.0, scalar2=cstt[:, C_PI:C_PI + 1], op0=ALU.mult, op1=ALU.add), rwf + [t_c], rwf)
        V(lambda e: e.tensor_tensor(out=chf, in0=pends[:, 31:32].to_broadcast([128, NBLK]), in1=cstt[:, C_BS:C_BS + NBLK], op=ALU.is_le), rwf + [t_c], rwf)
        V(lambda e: e.scalar_tensor_tensor(out=bef, in0=chf, scalar=1.0e6, in1=bef, op0=ALU.mult, op1=ALU.add), rwf, rwf)
        V(lambda e: e.tensor_copy(out=widx, in_=bef), rwf, [t_gd])
        tap("dest", dest, t_gd)
        tap("blkE", blkE, t_gd)
        tap("widx", widx, t_gd)
        tap("cum", cum, t_cum)
        _top = A.top
        A.release(m_persist)
        NWB = 3
        Wall = [A.alloc([12288], BF16) for _ in range(NWB)]
        A.top = _top
        t_wb = [Trk() for _ in range(NWB)]
        s_wb = [P.sem(f"s_wb{k_}") for k_ in range(NWB)]
        N_BCHK = 44

        def w_load(u):
            wb = u % NWB
            kw = dict(bounds_check=NE * 128 - 1, oob_is_err=False) if u >= NBLK - N_BCHK else {}
            P.op("gpsimd", lambda e: e.indirect_dma_start(out=Wall[wb], out_offset=None, in_=wbf,
                                                          in_offset=bass.IndirectOffsetOnAxis(ap=widx[:, u:u + 1], axis=0), **kw),
                 reads=[t_gd], writes=[t_wb[wb]], sem=s_wb[wb], amt=16)
        w_load(0)
        w_load(1)
        s_sc = P.sem("s_sc")

        def scatter(i, k_):
            P.op("gpsimd", lambda e: e.indirect_dma_start(out=xs_d, out_offset=bass.IndirectOffsetOnAxis(ap=dest[:, 2 * i + k_:2 * i + k_ + 1], axis=0), in_=F_all[:, i, :], in_offset=None),
                 reads=[t_F, t_gd], writes=[], sem=s_sc, amt=16)
        for i in range(n_st * 4):
            for k_ in range(2):
                scatter(i, k_)
        P.barrier()
        A.release(m_persist)

        Wall_ = [A.alloc([12288], BF16) for _ in range(NWB)]
        assert all(str(a_) == str(b_) for a_, b_ in zip(Wall, Wall_))
        Wg = [w_[:, 0:4096].rearrange("p (c n) -> p c n", c=8) for w_ in Wall]
        Wu = [w_[:, 4096:8192].rearrange("p (c n) -> p c n", c=8) for w_ in Wall]
        Wd = [w_[:, 8192:12288].rearrange("p (c n) -> p c n", c=4) for w_ in Wall]
        NXB = 4
        Xsb = [A.alloc([D], BF16) for _ in range(NXB)]
        t_xsb = [Trk() for _ in range(NXB)]
        s_xsb = [P.sem(f"s_xsb{k_}") for k_ in range(NXB)]
        XT = [A.alloc([8, 128], BF16) for _ in range(2)]
        t_XT = [Trk(), Trk()]
        gsb = A.alloc([512])
        hdn = A.alloc([512], BF16)
        hdnT = A.alloc([4, 128], BF16)
        ysb = [A.alloc([D]) for _ in range(2)]
        t_ysb = [Trk(), Trk()]
        s_y = [P.sem("s_y0"), P.sem("s_y1")]
        t_gsb, t_hdn, t_hdnT = Trk(), Trk(), Trk()
        NB = NBLK * UB if debug is None else debug.get("nblk", NBLK * UB)

        def X_load(b):
            xb = b % NXB
            P.dma("sync", Xsb[xb], xs_d[b * 128:(b + 1) * 128, :], s_xsb[xb], writes=[t_xsb[xb]])

        def S1(b):
            pb = b % 2
            xb = b % NXB
            tpv = bk_bf(0).rearrange("p (a b) -> p a b", b=128)
            for c in range(8):
                T(lambda e, c=c: e.transpose(tpv[:, c, :], Xsb[xb][:, c * 128:(c + 1) * 128], identb), [t_xsb[xb], t_c], [tbank[0]], inc=(c == 7))
            V(lambda e: e.tensor_copy(out=XT[pb], in_=tpv), [tbank[0]], [t_XT[pb]])

        def S2(b):
            pb = b % 2
            wb = (b // UB) % NWB
            for (bkk, W_) in ((2, Wg[wb]), (3, Wu[wb])):
                for c in range(8):
                    T(lambda e, c=c, bkk=bkk, W_=W_: e.matmul(banks[bkk][:, 0:512], lhsT=XT[pb][:, c, :], rhs=W_[:, c, :], start=(c == 0), stop=(c == 7)),
                      [t_XT[pb], t_wb[wb]], [tbank[bkk]], inc=(c == 7))
            Sc(lambda e: e.activation(out=gsb, in_=banks[2][:, 0:512], func=ACTF.Silu), [tbank[2]], [t_gsb])
            V(lambda e: e.tensor_tensor(out=hdn, in0=gsb, in1=banks[3][:, 0:512], op=ALU.mult), [t_gsb, tbank[3]], [t_hdn])

        def S3(b):
            tpv = bk_bf(1).rearrange("p (a b) -> p a b", b=128)
            for c in range(4):
                T(lambda e, c=c: e.transpose(tpv[:, c, :], hdn[:, c * 128:(c + 1) * 128], identb), [t_hdn, t_c], [tbank[1]], inc=(c == 3))
            Sc(lambda e: e.activation(out=hdnT, in_=tpv[:, 0:4, :], func=ACTF.Copy), [tbank[1]], [t_hdnT])

        def S4(b):
            pb = b % 2
            wb = (b // UB) % NWB
            for hf in range(2):
                bkk = 4 + hf
                for c in range(4):
                    T(lambda e, c=c, hf=hf, bkk=bkk: e.matmul(banks[bkk][:, 0:512], lhsT=hdnT[:, c, :], rhs=Wd[wb][:, c, hf * 512:(hf + 1) * 512], start=(c == 0), stop=(c == 3)),
                      [t_hdnT, t_wb[wb]], [tbank[bkk]], inc=(c == 3))
                if hf == 0:
                    Sc(lambda e: e.activation(out=ysb[pb][:, 0:512], in_=banks[4][:, 0:512], func=ACTF.Copy), [tbank[4]], [t_ysb[pb]])
                else:
                    V(lambda e: e.tensor_copy(out=ysb[pb][:, 512:1024], in_=banks[5][:, 0:512]), [tbank[5]], [t_ysb[pb]])
            P.dma("scalar", y_d[b * 128:(b + 1) * 128, :], ysb[pb], s_y[pb], reads=[t_ysb[pb]])

        NU = (NB + UB - 1) // UB
        for u0 in range(2, min(NWB, NU)):
            w_load(u0)
        for b0 in range(min(NXB, NB)):
            X_load(b0)
        S1(0)
        S2(0)
        if NB > 1:
            S1(1)
        for b in range(NB):
            S3(b)
            if b + 1 < NB:
                S2(b + 1)
            S4(b)
            if b % UB == UB - 1 and b // UB + NWB < NU:
                w_load(b // UB + NWB)
            if b + 2 < NB:
                S1(b + 2)
            if b + NXB < NB:
                X_load(b + NXB)
        P.barrier()
        A.release(m_persist)

        lnfin_b = A.alloc([D])
        t_lf = Trk()
        s_lf = P.sem("s_lf")
        P.dma("sync", lnfin_b, ln_final_w.partition_broadcast(128), s_lf, writes=[t_lf])
        N3 = 4
        hh = [A.alloc([D]) for _ in range(N3)]
        yg = [A.alloc([2, D]) for _ in range(N3)]
        ot = [A.alloc([D]) for _ in range(N3)]
        jk = A.alloc([D], BF16)
        sm3 = A.alloc([8])
        t_hh, t_yg, t_ot = [Trk() for _ in range(N3)], [[Trk(), Trk()] for _ in range(N3)], [Trk() for _ in range(N3)]
        s_hh = [P.sem(f"s_hh{k_}") for k_ in range(N3)]
        s_yg = [[P.sem(f"s_yg{k_}a"), P.sem(f"s_yg{k_}b")] for k_ in range(N3)]
        s_ot = [P.sem(f"s_ot{k_}") for k_ in range(N3)]
        t_jk, t_sm3 = Trk(), Trk()

        def fin_tile(i):
            pb = i % N3
            P.dma("sync", hh[pb], hbuf[i * 128:(i + 1) * 128, :], s_hh[pb], writes=[t_hh[pb]])
            for k_ in range(2):
                P.op("gpsimd", lambda e, k_=k_: e.indirect_dma_start(out=yg[pb][:, k_, :], out_offset=None, in_=y_d,
                                                                    in_offset=bass.IndirectOffsetOnAxis(ap=dest[:, 2 * i + k_:2 * i + k_ + 1], axis=0)),
                     reads=[t_gd], writes=[t_yg[pb][k_]], sem=s_yg[pb][k_], amt=16)
            acc = hh[pb]
            for k_ in range(2):
                V(lambda e, k_=k_: e.scalar_tensor_tensor(out=acc, in0=yg[pb][:, k_, :], scalar=gates[:, i, k_:k_ + 1], in1=acc, op0=ALU.mult, op1=ALU.add),
                  [t_yg[pb][k_], t_gd, t_hh[pb]], [t_hh[pb]])
            ss = sm3[:, 0:1]
            rstd = sm3[:, 1:2]
            Sc(lambda e: e.activation(out=jk, in_=acc, func=ACTF.Square, accum_out=ss), [t_hh[pb]], [t_jk, t_sm3])
            rstd_from_ss(ss, rstd, D, t_sm3)
            V(lambda e: e.scalar_tensor_tensor(out=ot[pb], in0=acc, scalar=rstd, in1=lnfin_b, op0=ALU.mult, op1=ALU.mult), [t_hh[pb], t_sm3, t_lf], [t_ot[pb]])
            P.dma("scalar", out[i * 128:(i + 1) * 128, :], ot[pb], s_ot[pb], reads=[t_ot[pb]])
        for i in range(n_st * 4):
            fin_tile(i)
        P.barrier()
        P.build()


def finish_copy(P, A, out, src, ntiles):
    buf = A.alloc([D])
    tb = Trk()
    s1 = P.sem("dbg_l")
    s2 = P.sem("dbg_s")
    ev = None
    for i in range(ntiles):
        P.dma("sync", buf, src[i * 128:(i + 1) * 128, :], s1, writes=[tb])
        ev = P.dma("sync", out[i * 128:(i + 1) * 128, :], buf, s2, reads=[tb])
    P.wait_events("sync", [(s2, s2.n, "dma")])


def make_consts():
    c = np.zeros((128, C_END), np.float32)
    k = np.arange(128)[:, None]
    q = np.arange(128)[None, :]
    c[:, C_ID:C_ID + 128] = np.eye(128, dtype=np.float32)
    c[:, C_CM:C_CM + 128] = np.where(k <= q, 0.0, NEG)
    c[:, C_PM:C_PM + 128] = np.where(k > q, 0.0, NEG)
    c[:, C_LT:C_LT + 128] = (k < q).astype(np.float32)
    c[:, C_ON:C_ON + 128] = 1.0
    inv = np.exp(-math.log(10000.0) * np.arange(0, 64, 2, dtype=np.float32) / 64).astype(np.float32)
    c[:, C_IF:C_IF + 32] = inv[None, :]
    c[:, C_IO:C_IO + 32] = np.arange(32, dtype=np.float32)[None, :]
    c[:, C_BS:C_BS + NBLK] = (np.arange(NBLK) * US).astype(np.float32)[None, :]
    c[:, C_PI] = np.arange(128, dtype=np.float32)
    return c


_W_NAMES = ["ln_mix_w", "w_in", "lambda_q1", "lambda_k1", "lambda_q2", "lambda_k2", "subln_w", "sinks", "w_out",
            "ln_cross_w", "ln_mem_w", "wq_cross", "wkv_cross", "wo_cross", "ln_moe_w", "w_group", "b_group",
            "w_expert", "b_expert", "w_gate", "w_up", "w_down"]


def make_in_maps(inputs, n=8):
    cst = make_consts()
    shared = {k: np.ascontiguousarray(np.asarray(inputs[k], dtype=np.float32)) for k in _W_NAMES}
    shared["ln_final_w"] = np.ascontiguousarray(np.asarray(inputs["ln_final_w"], dtype=np.float32).reshape(1, D))
    shared["cst"] = cst
    maps = []
    for b in range(n):
        m = dict(shared)
        m["x"] = np.ascontiguousarray(inputs["x"][b])
        m["mem"] = np.ascontiguousarray(inputs["mem"][b])
        m["post"] = np.ascontiguousarray(np.asarray(inputs["positions"][b], dtype=np.int32).reshape(NT, 128).T)
        maps.append(m)
    return maps


def kernel(**inputs):
    nc = bass.Bass("TRN2", target_bir_lowering=False)
    build_program(nc)
    maps = make_in_maps(inputs, 8)
    res = run_bass_kernel_spmd(nc, maps, core_ids=list(range(8)))
    return np.stack([np.asarray(r["out"]) for r in res.results], axis=0).astype(np.float32)
```

```python
import math
from contextlib import ExitStack
import numpy as np
import concourse.bass as bass
import concourse.mybir as mybir
from concourse.bass_utils import run_bass_kernel_spmd

F32 = mybir.dt.float32
BF16 = mybir.dt.bfloat16
I32 = mybir.dt.int32
ALU = mybir.AluOpType
ACTF = mybir.ActivationFunctionType
AX = mybir.AxisListType

ENGS = ("sync", "scalar", "vector", "gpsimd", "tensor")

S = 4096
D = 1024
NT = 32
NST = 8
INW = 2304
CAP = 512
NE = 32
UB = 2
US = 128 * UB
NUNIT = (8192 + 32 * (US - 1) + US - 1) // US
PS = NUNIT * US
NEG = -30000.0
EPS = 1e-6
TWO_PI = 2.0 * math.pi
C_ID, C_CM, C_PM, C_LT, C_ON, C_IF, C_IO, C_BS, C_PI, C_END = 0, 128, 256, 384, 512, 640, 672, 704, 800, 802
NBLK = NUNIT


class Sem:
    def __init__(self, h, name):
        self.h = h
        self.n = 0
        self.name = name


class Trk:
    __slots__ = ("w", "r")

    def __init__(self):
        self.w = None
        self.r = []


class Prog:
    def __init__(self, nc, stack):
        self.nc = nc
        self.stack = stack
        self.q = {e: [] for e in ENGS}
        self.sems = []
        self.esem = {e: self.sem("e_" + e) for e in ENGS if e != "sync"}
        self.waited = {e: {} for e in ENGS}

    def sem(self, name):
        h = self.stack.enter_context(self.nc.semaphore(name))
        s = Sem(h, name)
        self.sems.append(s)
        return s

    def sbuf(self, name, shape, dtype):
        return self.stack.enter_context(self.nc.sbuf_tensor(name, list(shape), dtype))

    def psum(self, name, shape, dtype):
        return self.stack.enter_context(self.nc.psum_tensor(name, list(shape), dtype))

    def _waits_for(self, eng, reads, writes):
        evs = []
        for t in reads:
            if t.w is not None:
                evs.append(t.w)
        for t in writes:
            if t.w is not None:
                evs.append(t.w)
            evs.extend(t.r)
        out = {}
        for (s, v, e) in evs:
            if e == eng and eng == "tensor":
                continue
            if self.waited[eng].get(s, 0) >= v:
                continue
            if out.get(s, 0) < v:
                out[s] = v
        for s, v in out.items():
            self.waited[eng][s] = v
        return list(out.items())

    def op(self, eng, fn, reads=(), writes=(), inc=True, sem=None, amt=1):
        waits = self._waits_for(eng, reads, writes)
        s = sem if sem is not None else self.esem[eng]
        if inc:
            s.n += amt
            val = s.n
        else:
            val = s.n + amt
        ev = (s, val, eng if sem is None else "dma")
        for t in reads:
            t.r.append(ev)
            if len(t.r) > 64:
                t.r = t.r[-64:]
        for t in writes:
            t.w = ev
            t.r = []
        sh = s.h

        def emit(e):
            for (ws, wv) in waits:
                e.wait_ge(ws.h, wv)
            ins = fn(e)
            if inc:
                ins.then_inc(sh, amt)
        self.q[eng].append(emit)
        return ev

    def dma(self, eng, out, in_, sem, reads=(), writes=(), **kw):
        return self.op(eng, lambda e: e.dma_start(out=out, in_=in_, **kw),
                       reads=reads, writes=writes, sem=sem, amt=16)

    def wait_events(self, eng, evs):
        ws = []
        for (s, v, e) in evs:
            if self.waited[eng].get(s, 0) >= v:
                continue
            self.waited[eng][s] = v
            ws.append((s, v))

        def emit(e):
            for (s, v) in ws:
                e.wait_ge(s.h, v)
        self.q[eng].append(emit)

    def barrier(self):
        evs = [(s, s.n, "x") for s in self.sems if s.n > 0]
        for e in ENGS:
            self.wait_events(e, evs)

    def build(self):
        with self.nc.Block() as block:
            for name in ENGS:
                fns = self.q[name]

                def body(e, fns=fns):
                    for f in fns:
                        f(e)
                getattr(block, name)(body)


class Arena:
    def __init__(self, P, words):
        self.t = P.sbuf("arena", [128, words], F32)
        self.top = 0
        self.words = words
        self.hi = 0

    def alloc(self, shape, dtype=F32):
        n = 1
        for s_ in shape:
            n *= s_
        esz = 4 if dtype in (F32, I32) else 2
        w = (n * esz + 3) // 4
        w = (w + 1) // 2 * 2
        ap = self.t[:, self.top:self.top + w]
        self.top += w
        self.hi = max(self.hi, self.top)
        assert self.top <= self.words, f"SBUF arena overflow {self.top} > {self.words}"
        if dtype != F32:
            ap = ap.bitcast(dtype)[:, 0:n]
        if len(shape) == 2:
            ap = ap.rearrange("p (a b) -> p a b", a=shape[0], b=shape[1])
        elif len(shape) == 3:
            ap = ap.rearrange("p (a b c) -> p a b c", a=shape[0], b=shape[1], c=shape[2])
        return ap

    def mark(self):
        return self.top

    def release(self, m):
        self.top = m


def bc(ap, axis, shape):
    return ap.unsqueeze(axis).to_broadcast(list(shape))


def build_program(nc, debug=None):
    dram = lambda name, shape, dt, kind="ExternalInput": nc.dram_tensor(name, list(shape), dt, kind=kind).ap()
    x = dram("x", [S, D], F32)
    mem = dram("mem", [256, D], F32)
    post = dram("post", [128, NT], I32)
    cst = dram("cst", [128, C_END], F32)
    ln_mix_w = dram("ln_mix_w", [1, D], F32)
    w_in = dram("w_in", [1, D, INW], F32)
    lq1 = dram("lambda_q1", [1, 64], F32)
    lk1 = dram("lambda_k1", [1, 64], F32)
    lq2 = dram("lambda_q2", [1, 64], F32)
    lk2 = dram("lambda_k2", [1, 64], F32)
    subln_w = dram("subln_w", [1, 128], F32)
    sinks = dram("sinks", [1, 8], F32)
    w_out = dram("w_out", [1, D, D], F32)
    ln_cross_w = dram("ln_cross_w", [1, D], F32)
    ln_mem_w = dram("ln_mem_w", [1, D], F32)
    wq_cross = dram("wq_cross", [1, D, D], F32)
    wkv_cross = dram("wkv_cross", [1, D, 2 * D], F32)
    wo_cross = dram("wo_cross", [1, D, D], F32)
    ln_moe_w = dram("ln_moe_w", [1, D], F32)
    w_group = dram("w_group", [1, D, 4], F32)
    b_group = dram("b_group", [1, 4], F32)
    w_expert = dram("w_expert", [1, D, 32], F32)
    b_expert = dram("b_expert", [1, 32], F32)
    w_gate = dram("w_gate", [1, NE, D, 512], F32)
    w_up = dram("w_up", [1, NE, D, 512], F32)
    w_down = dram("w_down", [1, NE, 512, D], F32)
    ln_final_w = dram("ln_final_w", [1, D], F32)
    out = dram("out", [S, D], F32, kind="ExternalOutput")
    hbuf = dram("hbuf", [S, D], F32, kind="Internal")
    xs_d = dram("xs_d", [PS + 128, D], BF16, kind="Internal")
    y_d = dram("y_d", [PS + 128, D], F32, kind="Internal")
    wbf = dram("wbf", [NE * 128, 12288], BF16, kind="Internal")

    with ExitStack() as st:
        P = Prog(nc, st)
        A = Arena(P, 52800)
        pp = [P.psum(f"pp{i}", [128, 1024], F32) for i in range(2)]
        banks = [pp[0][:, 0:512], pp[0][:, 512:1024], pp[1][:, 0:512], pp[1][:, 512:1024]] + [P.psum(f"bank{i}", [128, 512], F32)[:, :] for i in range(4, 8)]
        tbank = [Trk() for _ in range(8)]

        def bk_bf(i):
            return banks[i].bitcast(BF16)

        V = lambda fn, r=(), w=(): P.op("vector", fn, reads=r, writes=w)
        Sc = lambda fn, r=(), w=(): P.op("scalar", fn, reads=r, writes=w)
        G = lambda fn, r=(), w=(): P.op("gpsimd", fn, reads=r, writes=w)
        T = lambda fn, r=(), w=(), inc=True: P.op("tensor", fn, reads=r, writes=w, inc=inc)

        s_dbg = P.sem("s_dbg")
        taps = debug.get("taps", ()) if debug else ()

        def tap(name, ap, trk):
            if name not in taps:
                return
            shp = list(ap.shape)
            dt_ = nc.dram_tensor("dbg_" + name, shp, F32, kind="ExternalOutput").ap()
            P.dma("gpsimd", dt_, ap, s_dbg, reads=[trk])

        cstt = A.alloc([C_END])
        identf = cstt[:, C_ID:C_ID + 128]
        identb = A.alloc([128], BF16)
        cmask4 = A.alloc([512], BF16)
        pmask4 = A.alloc([512], BF16)
        ltrib = A.alloc([128], BF16)
        onesb = A.alloc([128], BF16)
        zerob = A.alloc([512], BF16)
        gates = A.alloc([NT, 2])
        dest = A.alloc([NT * 2], I32)
        blkE = A.alloc([NBLK], I32)
        widx = A.alloc([NBLK], I32)
        t_c = Trk()
        t_gd = Trk()
        s_c = P.sem("s_c")
        P.dma("sync", cstt, cst, s_c, writes=[t_c])
        V(lambda e: e.tensor_copy(out=identb, in_=cstt[:, C_ID:C_ID + 128]), [t_c], [t_c])
        V(lambda e: e.tensor_copy(out=cmask4.rearrange("p (a b) -> p a b", b=128), in_=bc(cstt[:, C_CM:C_CM + 128], 1, [128, 4, 128])), [t_c], [t_c])
        V(lambda e: e.tensor_copy(out=pmask4.rearrange("p (a b) -> p a b", b=128), in_=bc(cstt[:, C_PM:C_PM + 128], 1, [128, 4, 128])), [t_c], [t_c])
        V(lambda e: e.tensor_copy(out=ltrib, in_=cstt[:, C_LT:C_LT + 128]), [t_c], [t_c])
        V(lambda e: e.tensor_copy(out=onesb, in_=cstt[:, C_ON:C_ON + 128]), [t_c], [t_c])
        V(lambda e: e.memset(zerob, 0.0), [], [t_c])
        epsb = A.alloc([2])[:, 0:1]
        V(lambda e: e.memset(epsb, EPS), [], [t_c])
        m_persist = A.mark()

        def rstd_from_ss(ss, rstd, n, trk):
            Sc(lambda e: e.activation(out=rstd, in_=ss, func=ACTF.Sqrt, scale=1.0 / n, bias=epsb), [trk, t_c], [trk])
            V(lambda e: e.reciprocal(out=rstd, in_=rstd), [trk], [trk])

        Win = A.alloc([8, INW], BF16)
        Wout = A.alloc([8, D], BF16)
        KTd = A.alloc([4, S], BF16)
        Vda = A.alloc([NT, 4, 129], BF16)
        KTs = A.alloc([5 * 128], BF16)
        Vsa = A.alloc([5, 2, 65], BF16)
        cosT = A.alloc([NT, 32])
        sinT = A.alloc([NT, 32])
        smalls = A.alloc([512])
        lam_n = smalls[:, 0:1]
        esink = smalls[:, 8:16]
        subw = smalls[:, 128:256]
        t_w = Trk()
        t_kv = Trk()
        t_kvs = Trk()
        t_tab = Trk()
        s_w = P.sem("s_w")
        for hf in range(2):
            P.dma("gpsimd", Win[:, :, hf * 1152:(hf + 1) * 1152],
                  w_in[0][:, hf * 1152:(hf + 1) * 1152].rearrange("(c p) n -> p c n", p=128), s_w, writes=[t_w])
        P.dma("gpsimd", Wout, w_out[0].rearrange("(c p) n -> p c n", p=128), s_w, writes=[t_w])
        s_c3 = P.sem("s_c3")
        lnw8 = smalls[:, 384:392]
        P.dma("sync", lnw8, ln_mix_w.rearrange("o (c p) -> p (o c)", p=128), s_c3, writes=[t_tab], allow_slow_non_contiguous=True)
        for c_ in range(8):
            V(lambda e, c_=c_: e.tensor_scalar(out=Win[:, c_, :], in0=Win[:, c_, :], scalar1=lnw8[:, c_:c_ + 1], scalar2=None, op0=ALU.mult), [t_w, t_tab], [t_w])
        G(lambda e: e.memset(Vda, 1.0), [], [t_kv])
        G(lambda e: e.memset(Vsa, 1.0), [], [t_kvs])
        tm0 = A.mark()
        posi = A.alloc([NT], I32)
        posf = A.alloc([NT])
        ang = A.alloc([NT, 32])
        ki = A.alloc([NT, 32], I32)
        kf = A.alloc([NT, 32])
        rr = A.alloc([NT, 32])
        mm = A.alloc([NT, 32])
        lv = A.alloc([4, 64])
        t_t = Trk()
        s_c2 = P.sem("s_c2")
        P.dma("sync", posi, post, s_c2, writes=[t_t])
        for k_, src in enumerate((lq1, lk1, lq2, lk2)):
            P.dma("sync", lv[:, k_, :], src.partition_broadcast(128), s_c2, writes=[t_t])
        P.dma("sync", smalls[:, 16:24], sinks.partition_broadcast(128), s_c2, writes=[t_t])
        P.dma("sync", smalls[:, 256:384], subln_w.partition_broadcast(128), s_c2, writes=[t_t])
        V(lambda e: e.tensor_copy(out=posf, in_=posi), [t_t], [t_t])
        V(lambda e: e.tensor_tensor(out=ang, in0=bc(posf, 2, [128, NT, 32]), in1=bc(cstt[:, C_IF:C_IF + 32], 1, [128, NT, 32]), op=ALU.mult), [t_t, t_c], [t_t])

        def sin_of(dst, shift):
            V(lambda e: e.tensor_scalar(out=kf, in0=ang, scalar1=shift, scalar2=1.0 / TWO_PI, op0=ALU.add, op1=ALU.mult), [t_t], [t_t])
            V(lambda e: e.tensor_copy(out=ki, in_=kf), [t_t], [t_t])
            V(lambda e: e.tensor_copy(out=kf, in_=ki), [t_t], [t_t])
            V(lambda e: e.scalar_tensor_tensor(out=rr, in0=kf, scalar=-TWO_PI, in1=ang, op0=ALU.mult, op1=ALU.add), [t_t], [t_t])
            if shift != 0.0:
                V(lambda e: e.tensor_scalar(out=rr, in0=rr, scalar1=shift, scalar2=None, op0=ALU.add), [t_t], [t_t])
            V(lambda e: e.tensor_scalar(out=mm, in0=rr, scalar1=math.pi, scalar2=TWO_PI, op0=ALU.is_gt, op1=ALU.mult), [t_t], [t_t])
            V(lambda e: e.tensor_tensor(out=rr, in0=rr, in1=mm, op=ALU.subtract), [t_t], [t_t])
            V(lambda e: e.tensor_scalar(out=mm, in0=rr, scalar1=-math.pi, scalar2=TWO_PI, op0=ALU.is_lt, op1=ALU.mult), [t_t], [t_t])
            V(lambda e: e.tensor_tensor(out=rr, in0=rr, in1=mm, op=ALU.add), [t_t], [t_t])
            Sc(lambda e: e.activation(out=dst, in_=rr, func=ACTF.Sin), [t_t], [t_t, t_tab])
        sin_of(sinT, 0.0)
        sin_of(cosT, math.pi / 2)
        for a_ in (0, 2):
            V(lambda e, a_=a_: e.tensor_tensor(out=lv[:, a_, :], in0=lv[:, a_, :], in1=lv[:, a_ + 1, :], op=ALU.mult), [t_t], [t_t])
            V(lambda e, a_=a_: e.tensor_reduce(out=smalls[:, 32 + a_ // 2:33 + a_ // 2], in_=lv[:, a_, :], axis=AX.X, op=ALU.add), [t_t], [t_t])
        Sc(lambda e: e.activation(out=smalls[:, 34:36], in_=smalls[:, 32:34], func=ACTF.Exp), [t_t], [t_t])
        V(lambda e: e.tensor_tensor(out=smalls[:, 36:37], in0=smalls[:, 35:36], in1=smalls[:, 34:35], op=ALU.subtract), [t_t], [t_t])
        V(lambda e: e.tensor_scalar(out=lam_n, in0=smalls[:, 36:37], scalar1=-0.2, scalar2=None, op0=ALU.add), [t_t], [t_t, t_tab])
        Sc(lambda e: e.activation(out=esink, in_=smalls[:, 16:24], func=ACTF.Exp), [t_t], [t_t, t_tab])
        V(lambda e: e.tensor_scalar(out=subw, in0=smalls[:, 256:384], scalar1=0.8, scalar2=None, op0=ALU.mult), [t_t], [t_t, t_tab])
        P.barrier()
        A.release(tm0)

        xt = [A.alloc([D]) for _ in range(2)]
        t_xt = [Trk(), Trk()]
        s_xt = [P.sem("s_xt0"), P.sem("s_xt1")]
        xs = A.alloc([D], BF16)
        junk = xs
        xsT = A.alloc([8, 128], BF16)
        proj = A.alloc([INW])
        tmpB = A.alloc([1664])
        roped2 = [A.alloc([1664], BF16) for _ in range(2)]
        QTd = A.alloc([4, 512], BF16)
        QTs = A.alloc([4, 512], BF16)
        NPT = 3
        PTp = [A.alloc([1024], BF16) for _ in range(NPT)]
        PT = [p_[:, 0:512] for p_ in PTp]
        t_PT = [Trk() for _ in range(NPT)]
        Oc2 = A.alloc([2, 4, 129])
        t_Oc2 = Trk()

        Oc = A.alloc([2, 4, 129])
        Os = A.alloc([8, 65])
        sm = A.alloc([64])
        od = tmpB[:, 0:512].rearrange("p (a b) -> p a b", b=128)
        od2 = tmpB[:, 512:1024].rearrange("p (a b) -> p a b", b=128)
        o_tok = A.alloc([4, D], BF16)
        oT = xsT
        h1t = proj[:, 0:D]
        s_h1 = P.sem("s_h1")
        t_xs, t_xsT, t_proj, t_tmpB, t_roped, t_QT, t_Oc, t_Os, t_sm, t_od, t_otok, t_oT, t_h1 = [Trk() for _ in range(13)]
        t_ss = Trk()
        Ocs = [Oc, Oc2]
        t_Ocs = [t_Oc, t_Oc2]
        t_projS, t_tmpBS, t_ropedS = Trk(), Trk(), Trk()
        t_roped2 = [Trk(), Trk()]
        t_ropedS2 = [Trk(), Trk()]
        pt_ctr = [0]

        t_wbf = Trk()
        s_wbf = P.sem("s_wbf")

        def stage_P1(I, js):
            i = 4 * I + js
            roped = roped2[js % 2]
            t_roped, t_ropedS = t_roped2[js % 2], t_ropedS2[js % 2]
            ntl = 4 * (NST if debug is None else debug.get("nst", NST))
            nzf = (PS + 128) // 128
            for zi in range(i * nzf // ntl, (i + 1) * nzf // ntl):
                P.dma("gpsimd", xs_d[zi * 128:(zi + 1) * 128, :].rearrange("p (a b) -> p a b", a=2), bc(zerob, 1, [128, 2, 512]), s_wbf, reads=[t_c])
            for ex_ in range(i * NE // ntl, (i + 1) * NE // ntl):
                P.dma("gpsimd", wbf[ex_ * 128:(ex_ + 1) * 128, 0:4096].rearrange("p (c n) -> p c n", c=8), w_gate[0, ex_].rearrange("(c p) n -> p c n", p=128), s_wbf)
                P.dma("gpsimd", wbf[ex_ * 128:(ex_ + 1) * 128, 4096:8192].rearrange("p (c n) -> p c n", c=8), w_up[0, ex_].rearrange("(c p) n -> p c n", p=128), s_wbf)
                P.dma("gpsimd", wbf[ex_ * 128:(ex_ + 1) * 128, 8192:12288].rearrange("p (c n) -> p c n", c=4), w_down[0, ex_].rearrange("(c p) n -> p c n", p=128), s_wbf)
            b = i % 2
            xtb = xt[b]
            P.dma("sync", xtb, x[i * 128:(i + 1) * 128, :], s_xt[b], writes=[t_xt[b]])
            ss = sm[:, 0:1]
            rstd = sm[:, 1:2]
            Sc(lambda e: e.activation(out=junk, in_=xtb, func=ACTF.Square, accum_out=ss), [t_xt[b]], [t_xs, t_ss])
            rstd_from_ss(ss, rstd, D, t_ss)
            V(lambda e: e.tensor_scalar(out=xs, in0=xtb, scalar1=rstd, scalar2=None, op0=ALU.mult), [t_xt[b], t_ss], [t_xs])
            tpv = bk_bf(0).rearrange("p (a b) -> p a b", b=128)
            for c in range(8):
                T(lambda e, c=c: e.transpose(tpv[:, c, :], xs[:, c * 128:(c + 1) * 128], identb), [t_xs, t_c], [tbank[0]], inc=(c == 7))
            Sc(lambda e: e.activation(out=xsT, in_=tpv, func=ACTF.Copy), [tbank[0]], [t_xsT])

        def stage_P2(I, js):
            i = 4 * I + js
            roped = roped2[js % 2]
            t_roped, t_ropedS = t_roped2[js % 2], t_ropedS2[js % 2]
            groups = [(0, 512), (512, 512), (1024, 512), (1536, 512), (2048, 256)]
            for gi, (c0, cw) in enumerate(groups):
                bkk = 2 + gi % 2
                for c in range(8):
                    T(lambda e, c=c, c0=c0, cw=cw, bkk=bkk: e.matmul(banks[bkk][:, 0:cw], lhsT=xsT[:, c, :], rhs=Win[:, c, c0:c0 + cw], start=(c == 0), stop=(c == 7)),
                      [t_xsT, t_w], [tbank[bkk]], inc=(c == 7))
                tpj = t_proj if gi < 3 else t_projS
                Sc(lambda e, c0=c0, cw=cw, bkk=bkk: e.activation(out=proj[:, c0:c0 + cw], in_=banks[bkk][:, 0:cw], func=ACTF.Copy), [tbank[bkk]], [tpj])
            Sc(lambda e: e.activation(out=Vda[:, i, :, 0:128], in_=proj[:, 1024:1536].rearrange("p (h d) -> p h d", h=4), func=ACTF.Copy), [t_proj], [t_kv])
            Sc(lambda e: e.activation(out=Vsa[:, 1 + js, :, 0:64], in_=proj[:, 2176:2304].rearrange("p (h d) -> p h d", h=2), func=ACTF.Copy), [t_projS], [t_kvs])

        def stage_P3(I, js):
            i = 4 * I + js
            roped = roped2[js % 2]
            t_roped, t_ropedS = t_roped2[js % 2], t_ropedS2[js % 2]
            cb = cosT[:, i, :]
            sb = sinT[:, i, :]
            for (opf, p0, t0, r0, U, t_proj_, t_tmpB_, t_roped_) in ((V, 0, 0, 0, 16, t_proj, t_tmpB, t_roped), (V, 1536, 1024, 1024, 10, t_projS, t_tmpBS, t_ropedS)):
                pv = proj[:, p0:p0 + U * 64].rearrange("p (u t f) -> p u t f", u=U, t=2, f=32)
                tv = tmpB[:, t0:t0 + U * 64].rearrange("p (u t f) -> p u t f", u=U, t=2, f=32)
                cb4 = cb.unsqueeze(1).unsqueeze(1).to_broadcast([128, U, 2, 32])
                sb4 = sb.unsqueeze(1).unsqueeze(1).to_broadcast([128, U, 2, 32])
                opf(lambda e, pv=pv, tv=tv, sb4=sb4: e.tensor_tensor(out=tv, in0=pv, in1=sb4, op=ALU.mult), [t_proj_, t_tab], [t_tmpB_])
                opf(lambda e, pv=pv, cb4=cb4: e.tensor_tensor(out=pv, in0=pv, in1=cb4, op=ALU.mult), [t_proj_, t_tab], [t_proj_])
                if U == 16:
                    parts = [(pv, tv, roped[:, 0:1024].rearrange("p (u t f) -> p u t f", u=16, t=2, f=32))]
                else:
                    pq = proj[:, 1536:2048].rearrange("p (k g t f) -> p k g t f", k=2, g=4, t=2, f=32)
                    tq = tmpB[:, 1024:1536].rearrange("p (k g t f) -> p k g t f", k=2, g=4, t=2, f=32)
                    rq = roped[:, 1024:1536].rearrange("p (g k t f) -> p k g t f", k=2, g=4, t=2, f=32)
                    pk = proj[:, 2048:2176].rearrange("p (u t f) -> p u t f", u=2, t=2, f=32)
                    tk = tmpB[:, 1536:1664].rearrange("p (u t f) -> p u t f", u=2, t=2, f=32)
                    rk = roped[:, 1536:1664].rearrange("p (u t f) -> p u t f", u=2, t=2, f=32)
                    parts = [(pq, tq, rq), (pk, tk, rk)]
                for (pp, tt, rv) in parts:
                    if len(pp.shape) == 5:
                        sl0 = (slice(None), slice(None), slice(None), 0, slice(None))
                        sl1 = (slice(None), slice(None), slice(None), 1, slice(None))
                    else:
                        sl0 = (slice(None), slice(None), 0, slice(None))
                        sl1 = (slice(None), slice(None), 1, slice(None))
                    opf(lambda e, pp=pp, tt=tt, rv=rv, sl0=sl0, sl1=sl1: e.tensor_tensor(out=rv[sl0], in0=pp[sl0], in1=tt[sl1], op=ALU.subtract), [t_proj_, t_tmpB_], [t_roped_])
                    opf(lambda e, pp=pp, tt=tt, rv=rv, sl0=sl0, sl1=sl1: e.tensor_tensor(out=rv[sl1], in0=pp[sl1], in1=tt[sl0], op=ALU.add), [t_proj_, t_tmpB_], [t_roped_])
            if i == 0:
                tap("xs", xs, t_xs)
                tap("xsT", xsT, t_xsT)
                tap("proj", proj, t_projS)
                tap("roped", roped, t_ropedS)
                tap("ropedD", roped[:, 0:1024], t_roped)
                tap("cos", cosT, t_tab)
                tap("sin", sinT, t_tab)
                tap("sm", sm, t_ss)

        def stage_P4(I, js):
            i = 4 * I + js
            roped = roped2[js % 2]
            t_roped, t_ropedS = t_roped2[js % 2], t_ropedS2[js % 2]
            tp0 = bk_bf(0).rearrange("p (a b) -> p a b", b=128)
            tp1 = bk_bf(1).rearrange("p (a b) -> p a b", b=128)
            for h in range(4):
                T(lambda e, h=h: e.transpose(tp0[:, h, :], roped[:, h * 128:(h + 1) * 128], identb), [t_roped, t_c], [tbank[0]], inc=False)
            for h in range(4):
                T(lambda e, h=h: e.transpose(tp0[:, 4 + h, :], roped[:, 512 + h * 128:512 + (h + 1) * 128], identb), [t_roped, t_c], [tbank[0]], inc=(h == 3))
            for g in range(4):
                T(lambda e, g=g: e.transpose(tp1[:, g, :], roped[:, 1024 + g * 128:1024 + (g + 1) * 128], identb), [t_ropedS, t_c], [tbank[1]], inc=False)
            T(lambda e: e.transpose(tp1[:, 4, :], roped[:, 1536:1664], identb), [t_ropedS, t_c], [tbank[1]])
            Sc(lambda e: e.activation(out=QTd[:, :, js * 128:(js + 1) * 128], in_=tp0[:, 0:4, :], func=ACTF.Copy), [tbank[0]], [t_QT])
            Sc(lambda e: e.activation(out=KTd[:, :, i * 128:(i + 1) * 128], in_=tp0[:, 4:8, :], func=ACTF.Copy), [tbank[0]], [t_kv])
            Sc(lambda e: e.activation(out=QTs[:, js, :], in_=bk_bf(1)[:, 0:512], func=ACTF.Copy), [tbank[1]], [t_QT])
            Sc(lambda e: e.activation(out=KTs[:, (1 + js) * 128:(2 + js) * 128], in_=tp1[:, 4, :], func=ACTF.Copy), [tbank[1]], [t_kvs])

        def stage_E(I):
            blocks = []
            L = 4 * I + 4
            for h in range(4):
                for kt in range(L):
                    for c in range(2):
                        blocks.append((h, c, kt, kt == 0, kt == L - 1))
            NB = len(blocks)

            def emit_S(n):
                h, c, kt, first, last = blocks[n]
                qlo = max(0, kt - 4 * I)
                bkk = n % 4
                diag = kt >= 4 * I
                T(lambda e: e.matmul(banks[bkk][:, qlo * 128:512], lhsT=KTd[c * 64:(c + 1) * 64, h, kt * 128:(kt + 1) * 128],
                                     rhs=QTd[c * 64:(c + 1) * 64, h, qlo * 128:512], start=True, stop=not diag),
                  [t_kv, t_QT], [tbank[bkk]], inc=not diag)
                if diag:
                    T(lambda e: e.matmul(banks[bkk][:, qlo * 128:(qlo + 1) * 128], lhsT=identb, rhs=cmask4[:, 0:128], start=False, stop=True),
                      [t_c], [tbank[bkk]])

            def emit_EXP(p):
                n = 2 * p
                h, c, kt, first, last = blocks[n]
                qlo = max(0, kt - 4 * I)
                k = pt_ctr[0] % NPT
                pt_ctr[0] += 1
                src = pp[p % 2].rearrange("p (c q) -> p c q", c=2)[:, :, qlo * 128:512]
                dst = PTp[k].rearrange("p (c q) -> p c q", c=2)[:, :, qlo * 128:512]
                Sc(lambda e: e.activation(out=dst, in_=src, func=ACTF.Exp, scale=0.125), [tbank[n % 4], tbank[(n + 1) % 4]], [t_PT[k]])
                return k

            def emit_PV(n, k):
                h, c, kt, first, last = blocks[n]
                qlo = max(0, kt - 4 * I)
                abs_ = (4 + 2 * c, 5 + 2 * c)
                if first:
                    for ab in abs_:
                        T(lambda e, ab=ab: e.matmul(banks[ab][:, 0:258], lhsT=zerob[0:1, 0:128], rhs=zerob[0:1, 0:258], start=True, stop=False, skip_group_check=True),
                          [t_c], [tbank[ab]], inc=False)
                for jq in range(qlo, 4):
                    ab = abs_[jq // 2]
                    av = banks[ab][:, 0:258].rearrange("p (a b) -> p a b", b=129)
                    T(lambda e, jq=jq, av=av: e.matmul(av[:, jq % 2, :], lhsT=PTp[k][:, c * 512 + jq * 128:c * 512 + (jq + 1) * 128], rhs=Vda[:, kt, h, :], start=False, stop=(kt == 4 * I + jq), skip_group_check=True),
                      [t_PT[k], t_kv], [tbank[ab]], inc=(jq == 3))
                if last:
                    for ai, ab in enumerate(abs_):
                        av = banks[ab][:, 0:258].rearrange("p (a b) -> p a b", b=129)
                        V(lambda e, ai=ai, av=av: e.tensor_copy(out=Ocs[h % 2][:, c, 2 * ai:2 * ai + 2, :], in_=av), [tbank[ab]], [t_Ocs[h % 2]])
                    if c == 1:
                        pend_fin.append((h, n // 2 + 5))

            def finalize_diff(h):
                rs = sm[:, 8:16].rearrange("p (c j) -> p c j", c=2)
                V(lambda e: e.reciprocal(out=rs, in_=Ocs[h % 2][:, :, :, 128]), [t_Ocs[h % 2]], [t_sm])
                V(lambda e: e.tensor_scalar(out=rs[:, 1, :], in0=rs[:, 1, :], scalar1=lam_n, scalar2=None, op0=ALU.mult), [t_sm, t_tab], [t_sm])
                V(lambda e: e.tensor_tensor(out=od, in0=Ocs[h % 2][:, 0, :, 0:128], in1=bc(rs[:, 0, :], 2, [128, 4, 128]), op=ALU.mult), [t_Ocs[h % 2], t_sm], [t_od])
                V(lambda e: e.tensor_tensor(out=od2, in0=Ocs[h % 2][:, 1, :, 0:128], in1=bc(rs[:, 1, :], 2, [128, 4, 128]), op=ALU.mult), [t_Ocs[h % 2], t_sm], [t_od])
                V(lambda e: e.tensor_tensor(out=od, in0=od, in1=od2, op=ALU.add), [t_od], [t_od])
                V(lambda e: e.tensor_tensor(out=od2, in0=od, in1=od, op=ALU.mult), [t_od], [t_od])
                ssd = sm[:, 16:20]
                V(lambda e: e.tensor_reduce(out=ssd, in_=od2, axis=AX.X, op=ALU.add), [t_od], [t_sm])
                rstd_from_ss(ssd, ssd, 128, t_sm)
                V(lambda e: e.tensor_tensor(out=od, in0=od, in1=bc(ssd, 2, [128, 4, 128]), op=ALU.mult), [t_od, t_sm], [t_od])
                V(lambda e: e.tensor_tensor(out=o_tok[:, :, h * 128:(h + 1) * 128], in0=od, in1=bc(subw, 1, [128, 4, 128]), op=ALU.mult), [t_od, t_tab], [t_otok])

            pend_fin = []
            for n in range(min(4, NB)):
                emit_S(n)
            NPr = NB // 2
            for p in range(NPr):
                k = emit_EXP(p)
                emit_PV(2 * p, k)
                emit_PV(2 * p + 1, k)
                if 2 * p + 4 < NB:
                    emit_S(2 * p + 4)
                    emit_S(2 * p + 5)
                while pend_fin and pend_fin[0][1] <= p:
                    finalize_diff(pend_fin.pop(0)[0])
            while pend_fin:
                finalize_diff(pend_fin.pop(0)[0])

        PTh = [PTp[k_][:, hh_ * 512:(hh_ + 1) * 512] for k_ in range(NPT) for hh_ in range(2)]
        t_PTh = [Trk() for _ in range(2 * NPT)]

        def swa_blocks(I, js):
            i = 4 * I + js
            out_ = []
            for kv in range(2):
                kts = ([(js, pmask4)] if i > 0 else []) + [(js + 1, cmask4)]
                for ti, (lb, mk) in enumerate(kts):
                    out_.append((kv, ti, lb, mk, len(kts)))
            return out_

        def swa_S(I, js):
            for bi, (kv, ti, lb, mk, nk) in enumerate(swa_blocks(I, js)):
                swa_S1(I, js, bi, kv, lb, mk)

        def swa_S1(I, js, bi, kv, lb, mk):
            sv = banks[bi][:, 0:512]
            T(lambda e: e.matmul(sv, lhsT=KTs[kv * 64:(kv + 1) * 64, lb * 128:(lb + 1) * 128],
                                 rhs=QTs[kv * 64:(kv + 1) * 64, js, :], start=True, stop=False),
              [t_kvs, t_QT], [tbank[bi]], inc=False)
            T(lambda e: e.matmul(sv, lhsT=identb, rhs=mk, start=False, stop=True), [t_c], [tbank[bi]])

        def swa_EXP(I, js):
            for bi, (kv, ti, lb, mk, nk) in enumerate(swa_blocks(I, js)):
                Sc(lambda e, bi=bi: e.activation(out=PTh[bi], in_=banks[bi][:, 0:512], func=ACTF.Exp, scale=0.125), [tbank[bi]], [t_PTh[bi], t_PT[bi // 2]])

        def swa_PV(I, js):
            for kv in range(2):
                ab = 6 + kv
                T(lambda e, ab=ab: e.matmul(banks[ab][:, 0:260], lhsT=zerob[0:1, 0:128], rhs=zerob[0:1, 0:260], start=True, stop=False, skip_group_check=True),
                  [t_c], [tbank[ab]], inc=False)
            for bi, (kv, ti, lb, mk, nk) in enumerate(swa_blocks(I, js)):
                swa_PV1(I, js, bi, kv, ti, lb, nk)

        def swa_PV1(I, js, bi, kv, ti, lb, nk):
            ab = 6 + kv
            av = banks[ab][:, 0:260].rearrange("p (a b) -> p a b", b=65)
            for g in range(4):
                T(lambda e, g=g: e.matmul(av[:, g, :], lhsT=PTh[bi][:, g * 128:(g + 1) * 128], rhs=Vsa[:, lb, kv, :], start=False,
                                          stop=(ti == nk - 1), skip_group_check=True),
                  [t_PTh[bi], t_PT[bi // 2], t_kvs], [tbank[ab]], inc=(g == 3))

        def swa_FIN(I, js):
            for kv in range(2):
                ab = 6 + kv
                av = banks[ab][:, 0:260].rearrange("p (a b) -> p a b", b=65)
                V(lambda e, av=av, kv=kv: e.tensor_copy(out=Os[:, kv * 4:(kv + 1) * 4, :], in_=av), [tbank[ab]], [t_Os])
            den = sm[:, 24:32]
            V(lambda e: e.tensor_tensor(out=den, in0=Os[:, :, 64], in1=esink, op=ALU.add), [t_Os, t_tab], [t_sm])
            V(lambda e: e.reciprocal(out=den, in_=den), [t_sm], [t_sm])
            V(lambda e: e.tensor_tensor(out=o_tok[:, js, 512:1024].rearrange("p (h d) -> p h d", h=8), in0=Os[:, :, 0:64], in1=bc(den, 2, [128, 8, 64]), op=ALU.mult),
              [t_Os, t_sm], [t_otok])

        def stage_F(I):
            swa_S(I, 0)
            swa_EXP(I, 0)
            for js in range(4):
                swa_PV(I, js)
                if js + 1 < 4:
                    swa_S(I, js + 1)
                swa_FIN(I, js)
                if js + 1 < 4:
                    swa_EXP(I, js + 1)
            V(lambda e: e.tensor_copy(out=KTs[:, 0:128], in_=KTs[:, 512:640]), [t_kvs], [t_kvs])
            V(lambda e: e.tensor_copy(out=Vsa[:, 0, :, :], in_=Vsa[:, 4, :, :]), [t_kvs], [t_kvs])

        oT2 = [xsT, xs.rearrange("p (a b) -> p a b", b=128)]
        t_oT2 = [t_xsT, t_xs]

        def H1(I, js):
            i = 4 * I + js
            x2t = xt[js % 2]
            t_x2 = t_xt[js % 2]
            P.dma("sync", x2t, x[i * 128:(i + 1) * 128, :], s_xt[js % 2], writes=[t_x2])
            tpv = bk_bf(1).rearrange("p (a b) -> p a b", b=128)
            for c in range(8):
                T(lambda e, c=c: e.transpose(tpv[:, c, :], o_tok[:, js, c * 128:(c + 1) * 128], identb), [t_otok, t_c], [tbank[1]], inc=(c == 7))
            V(lambda e: e.tensor_copy(out=oT2[js % 2], in_=tpv), [tbank[1]], [t_oT2[js % 2]])

        def H2(I, js):
            i = 4 * I + js
            x2t = xt[js % 2]
            t_x2 = t_xt[js % 2]
            oT_, t_oT_ = oT2[js % 2], t_oT2[js % 2]
            for hf in range(2):
                bkk = 2 + hf
                for c in range(8):
                    T(lambda e, c=c, hf=hf, bkk=bkk: e.matmul(banks[bkk][:, 0:512], lhsT=oT_[:, c, :], rhs=Wout[:, c, hf * 512:(hf + 1) * 512], start=(c == 0), stop=(c == 7)),
                      [t_oT_, t_w], [tbank[bkk]], inc=(c == 7))
                V(lambda e, hf=hf, bkk=bkk: e.tensor_tensor(out=h1t[:, hf * 512:(hf + 1) * 512], in0=banks[bkk][:, 0:512], in1=x2t[:, hf * 512:(hf + 1) * 512], op=ALU.add),
                  [tbank[bkk], t_x2], [t_h1, t_proj])
            P.dma("scalar", hbuf[i * 128:(i + 1) * 128, :], h1t, s_h1, reads=[t_h1, t_proj])

        def stage_H(I):
            H1(I, 0)
            for js in range(4):
                if js + 1 < 4:
                    H1(I, js + 1)
                H2(I, js)

        n_st = NST if debug is None else debug.get("nst", NST)
        for I in range(n_st):
            stage_P1(I, 0)
            stage_P2(I, 0)
            for js in range(4):
                if js + 1 < 4:
                    stage_P1(I, js + 1)
                stage_P3(I, js)
                if js + 1 < 4:
                    stage_P2(I, js + 1)
                stage_P4(I, js)
            if I == 0:
                tap("QTd", QTd, t_QT)
                tap("QTs", QTs, t_QT)
                tap("KTd", KTd[:, :, 0:512], t_kv)
                tap("Vda", Vda[:, 0:4, :, :], t_kv)
            stage_E(I)
            stage_F(I)
            if I == 0:
                tap("otok", o_tok, t_otok)
                tap("Oc", Oc, t_Oc)
            stage_H(I)

        if debug is not None and debug.get("stop") == "1A":
            P.barrier()
            finish_copy(P, A, out, hbuf, n_st * 4)
            P.build()
            return
        P.barrier()
        A.release(m_persist)

        Wq = A.alloc([8, D], BF16)
        Wo = A.alloc([8, D], BF16)
        KcT = A.alloc([8, 256], BF16)
        Vca = A.alloc([2, 4, 257], BF16)
        lncross_b = A.alloc([D])
        lnmoe_b = A.alloc([D])
        bias_b = A.alloc([36])
        Wr = A.alloc([8, 36])
        t_w2, t_kc, t_tab2 = Trk(), Trk(), Trk()
        s_w2 = P.sem("s_w2")
        s_t2 = P.sem("s_t2")
        P.dma("gpsimd", Wq, wq_cross[0].rearrange("(c p) n -> p c n", p=128), s_w2, writes=[t_w2])
        P.dma("gpsimd", Wo, wo_cross[0].rearrange("(c p) n -> p c n", p=128), s_w2, writes=[t_w2])
        P.dma("sync", lncross_b, ln_cross_w.partition_broadcast(128), s_t2, writes=[t_tab2])
        P.dma("sync", lnmoe_b, ln_moe_w.partition_broadcast(128), s_t2, writes=[t_tab2])
        P.dma("sync", bias_b[:, 0:4], b_group.partition_broadcast(128), s_t2, writes=[t_tab2])
        P.dma("sync", bias_b[:, 4:36], b_expert.partition_broadcast(128), s_t2, writes=[t_tab2])
        P.dma("sync", Wr[:, :, 0:4], w_group[0].rearrange("(c p) n -> p c n", p=128), s_t2, writes=[t_tab2])
        P.dma("sync", Wr[:, :, 4:36], w_expert[0].rearrange("(c p) n -> p c n", p=128), s_t2, writes=[t_tab2])
        G(lambda e: e.memset(Vca, 1.0), [], [t_kc])
        sm2 = A.alloc([64])
        t_sm2 = Trk()
        m1b = A.mark()
        Wkv = A.alloc([8, 2 * D], BF16)
        memt = A.alloc([2, D])
        lnmem_b = A.alloc([D])
        msb = A.alloc([D], BF16)
        msT = A.alloc([8, 256], BF16)
        t_m, t_ms, t_msT, t_mw = Trk(), Trk(), Trk(), Trk()
        s_m = P.sem("s_m")
        s_m2 = P.sem("s_m2")
        P.dma("gpsimd", Wkv, wkv_cross[0].rearrange("(c p) n -> p c n", p=128), s_m2, writes=[t_mw])
        P.dma("sync", memt, mem.rearrange("(t p) d -> p t d", p=128), s_m, writes=[t_m])
        P.dma("sync", lnmem_b, ln_mem_w.partition_broadcast(128), s_m, writes=[t_m])

        def mem_tile(mt):
            ss = sm2[:, 0:1]
            rstd = sm2[:, 1:2]
            Sc(lambda e: e.activation(out=msb, in_=memt[:, mt, :], func=ACTF.Square, accum_out=ss), [t_m], [t_ms, t_sm2])
            rstd_from_ss(ss, rstd, D, t_sm2)
            V(lambda e: e.scalar_tensor_tensor(out=msb, in0=memt[:, mt, :], scalar=rstd, in1=lnmem_b, op0=ALU.mult, op1=ALU.mult), [t_m, t_sm2], [t_ms])
            tpv = bk_bf(0).rearrange("p (a b) -> p a b", b=128)
            for c in range(8):
                T(lambda e, c=c: e.transpose(tpv[:, c, :], msb[:, c * 128:(c + 1) * 128], identb), [t_ms, t_c], [tbank[0]], inc=(c == 7))
            V(lambda e: e.tensor_copy(out=msT[:, :, mt * 128:(mt + 1) * 128], in_=tpv), [tbank[0]], [t_msT])
        for mt in range(2):
            mem_tile(mt)

        def kc_chunk(fc):
            bkk = 2 + fc % 2
            for c in range(8):
                T(lambda e, c=c: e.matmul(banks[bkk][:, 0:256], lhsT=Wkv[:, c, fc * 128:(fc + 1) * 128], rhs=msT[:, c, :], start=(c == 0), stop=(c == 7)),
                  [t_mw, t_msT], [tbank[bkk]], inc=(c == 7))
            V(lambda e: e.tensor_copy(out=KcT[:, fc, :], in_=banks[bkk][:, 0:256]), [tbank[bkk]], [t_kc])
        for fc in range(8):
            kc_chunk(fc)

        def vc_chunk(mt, hf):
            bkk = 2 + hf
            for c in range(8):
                T(lambda e, c=c: e.matmul(banks[bkk][:, 0:512], lhsT=msT[:, c, mt * 128:(mt + 1) * 128], rhs=Wkv[:, c, D + hf * 512:D + (hf + 1) * 512], start=(c == 0), stop=(c == 7)),
                  [t_mw, t_msT], [tbank[bkk]], inc=(c == 7))
            Sc(lambda e: e.activation(out=Vca[:, mt, 2 * hf:2 * hf + 2, 0:256], in_=banks[bkk][:, 0:512].rearrange("p (a b) -> p a b", b=256), func=ACTF.Copy), [tbank[bkk]], [t_kc])
        for mt in range(2):
            for hf in range(2):
                vc_chunk(mt, hf)
        P.barrier()
        A.release(m1b)

        F_all = A.alloc([NT, D], BF16)
        Lg_all = A.alloc([NT, 36])
        eidf = A.alloc([NT * 2])
        posv = A.alloc([NT * 2])
        cum = A.alloc([32])
        m_loop = A.mark()
        ht2 = [A.alloc([4, D]) for _ in range(2)]
        t_ht2 = [[Trk() for _ in range(4)] for _ in range(2)]
        s_ht2 = [[P.sem(f"s_ht{q}{j}") for j in range(4)] for q in range(2)]
        s_h22 = [[P.sem(f"s_h2{q}{j}") for j in range(4)] for q in range(2)]
        hs = A.alloc([D], BF16)
        hs_r = A.alloc([D], BF16)
        hsT_ = A.alloc([8, 512], BF16)
        hsT2 = [hsT_, hsT_]
        t_hsT_ = Trk()
        t_hsT2 = [t_hsT_, t_hsT_]
        t_hsr, t_smL, t_smR, t_smP = Trk(), Trk(), Trk(), Trk()
        Ohc = A.alloc([4, 257])
        t_Ohc = Trk()
        qcT = A.alloc([8, 512], BF16)
        PcT = [A.alloc([512], BF16) for _ in range(4)]
        t_PcT = [Trk() for _ in range(4)]
        oc_tok = A.alloc([4, D], BF16)
        ocT = A.alloc([8, 128], BF16)
        hm = A.alloc([D])
        hmT = A.alloc([8, 128])
        t_hs, t_hsT, t_qcT, t_octok, t_ocT, t_hm, t_hmT, t_L, t_rt, t_F, t_cum, t_Mb = [Trk() for _ in range(12)]
        V(lambda e: e.tensor_copy(out=Lg_all[:, :, 0:4], in_=bc(cstt[:, C_IO:C_IO + 4], 1, [128, NT, 4])), [t_c], [t_L])
        V(lambda e: e.tensor_copy(out=Lg_all[:, :, 4:36], in_=bc(cstt[:, C_IO:C_IO + 32], 1, [128, NT, 32])), [t_c], [t_L])
        pc_ctr = [0]
        iota_e = cstt[:, C_IO:C_IO + 32]

        def b_load_a(I, js):
            i = 4 * I + js
            ht, t_ht, s_ht = ht2[I % 2], t_ht2[I % 2], s_ht2[I % 2]
            P.dma("sync", ht[:, js, :], hbuf[i * 128:(i + 1) * 128, :], s_ht[js], writes=[t_ht[js]])
            ss = sm2[:, 0:1]
            rstd = sm2[:, 1:2]
            Sc(lambda e: e.activation(out=hs, in_=ht[:, js, :], func=ACTF.Square, accum_out=ss), [t_ht[js]], [t_hs, t_smL])
            rstd_from_ss(ss, rstd, D, t_smL)
            V(lambda e: e.scalar_tensor_tensor(out=hs, in0=ht[:, js, :], scalar=rstd, in1=lncross_b, op0=ALU.mult, op1=ALU.mult), [t_ht[js], t_smL, t_tab2], [t_hs])

        def b_load_b(I, js):
            hsT, t_hsT = hsT2[I % 2], t_hsT2[I % 2]
            tpv = bk_bf(0).rearrange("p (a b) -> p a b", b=128)
            for c in range(8):
                T(lambda e, c=c: e.transpose(tpv[:, c, :], hs[:, c * 128:(c + 1) * 128], identb), [t_hs, t_c], [tbank[0]], inc=(c == 7))
            V(lambda e: e.tensor_copy(out=hsT[:, :, js * 128:(js + 1) * 128], in_=tpv), [tbank[0]], [t_hsT])

        def b_qproj(I, fc, bb=2):
            hsT, t_hsT = hsT2[I % 2], t_hsT2[I % 2]
            bkk = bb + fc % 2
            for c in range(8):
                T(lambda e, c=c: e.matmul(banks[bkk][:, 0:512], lhsT=Wq[:, c, fc * 128:(fc + 1) * 128], rhs=hsT[:, c, :], start=(c == 0), stop=(c == 7)),
                  [t_w2, t_hsT], [tbank[bkk]], inc=(c == 7))
            if fc % 2 == 0:
                Sc(lambda e: e.activation(out=qcT[:, fc, :], in_=banks[bkk][:, 0:512], func=ACTF.Copy), [tbank[bkk]], [t_qcT])
            else:
                V(lambda e: e.tensor_copy(out=qcT[:, fc, :], in_=banks[bkk][:, 0:512]), [tbank[bkk]], [t_qcT])

        def b_score(hc, mt):
            bkk = 4 + mt
            for u in range(2):
                T(lambda e, u=u: e.matmul(banks[bkk][:, 0:512], lhsT=KcT[:, 2 * hc + u, mt * 128:(mt + 1) * 128], rhs=qcT[:, 2 * hc + u, :], start=(u == 0), stop=(u == 1)),
                  [t_kc, t_qcT], [tbank[bkk]], inc=(u == 1))
            k = pc_ctr[0] % 4
            pc_ctr[0] += 1
            Sc(lambda e: e.activation(out=PcT[k], in_=banks[bkk][:, 0:512], func=ACTF.Exp, scale=1.0 / 16.0), [tbank[bkk]], [t_PcT[k]])
            return k

        def b_pv(hc, js, ks):
            ab = 6 + js % 2
            for mt in range(2):
                T(lambda e, mt=mt: e.matmul(banks[ab][:, 0:257], lhsT=PcT[ks[mt]][:, js * 128:(js + 1) * 128], rhs=Vca[:, mt, hc, :], start=(mt == 0), stop=(mt == 1)),
                  [t_PcT[ks[mt]], t_kc], [tbank[ab]], inc=(mt == 1))
            Sc(lambda e: e.activation(out=Ohc[:, js, :], in_=banks[ab][:, 0:257], func=ACTF.Copy), [tbank[ab]], [t_Ohc])
            if js == 3:
                rd = sm2[:, 4:8]
                V(lambda e: e.reciprocal(out=rd, in_=Ohc[:, :, 256]), [t_Ohc], [t_smP])
                V(lambda e: e.tensor_tensor(out=oc_tok[:, :, hc * 256:(hc + 1) * 256], in0=Ohc[:, :, 0:256], in1=bc(rd, 2, [128, 4, 256]), op=ALU.mult), [t_Ohc, t_smP], [t_octok])

        def b_O1(I, js):
            tpv = bk_bf(1).rearrange("p (a b) -> p a b", b=128)
            for c in range(8):
                T(lambda e, c=c: e.transpose(tpv[:, c, :], oc_tok[:, js, c * 128:(c + 1) * 128], identb), [t_octok, t_c], [tbank[1]], inc=(c == 7))
            V(lambda e: e.tensor_copy(out=ocT, in_=tpv), [tbank[1]], [t_ocT])

        def b_O2(I, js):
            i = 4 * I + js
            ht, t_ht, s_h2 = ht2[I % 2], t_ht2[I % 2], s_h22[I % 2]
            for hf in range(2):
                bkk = 2 + hf
                for c in range(8):
                    T(lambda e, c=c, hf=hf, bkk=bkk: e.matmul(banks[bkk][:, 0:512], lhsT=ocT[:, c, :], rhs=Wo[:, c, hf * 512:(hf + 1) * 512], start=(c == 0), stop=(c == 7)),
                      [t_ocT, t_w2], [tbank[bkk]], inc=(c == 7))
                V(lambda e, hf=hf, bkk=bkk: e.tensor_tensor(out=ht[:, js, hf * 512:(hf + 1) * 512], in0=banks[bkk][:, 0:512], in1=ht[:, js, hf * 512:(hf + 1) * 512], op=ALU.add),
                  [tbank[bkk], t_ht[js]], [t_ht[js]])
            P.dma("scalar", hbuf[i * 128:(i + 1) * 128, :], ht[:, js, :], s_h2[js], reads=[t_ht[js]])

        def b_R1(I, js):
            i = 4 * I + js
            ht, t_ht = ht2[I % 2], t_ht2[I % 2]
            h2 = ht[:, js, :]
            ss = sm2[:, 2:3]
            rstd = sm2[:, 3:4]
            V(lambda e: e.tensor_tensor(out=hm, in0=h2, in1=lnmoe_b, op=ALU.mult), [t_ht[js], t_tab2], [t_hm])
            Sc(lambda e: e.activation(out=hs_r, in_=h2, func=ACTF.Square, accum_out=ss), [t_ht[js]], [t_hsr, t_smR])
            rstd_from_ss(ss, rstd, D, t_smR)
            Sc(lambda e: e.activation(out=F_all[:, i, :], in_=hm, func=ACTF.Copy, scale=rstd), [t_hm, t_smR], [t_F])
            for half in range(2):
                bkk = 6 + half
                for c4 in range(4):
                    c = half * 4 + c4
                    T(lambda e, c=c, c4=c4, bkk=bkk: e.transpose(banks[bkk][:, c4 * 128:(c4 + 1) * 128], hm[:, c * 128:(c + 1) * 128], identf), [t_hm, t_c], [tbank[bkk]], inc=(c4 == 3))
                if half == 0:
                    V(lambda e: e.tensor_copy(out=hmT[:, 0:4, :], in_=banks[6][:, 0:512].rearrange("p (a b) -> p a b", b=128)), [tbank[6]], [t_hmT])
                else:
                    Sc(lambda e: e.activation(out=hmT[:, 4:8, :], in_=banks[7][:, 0:512].rearrange("p (a b) -> p a b", b=128), func=ACTF.Copy), [tbank[7]], [t_hmT])

        def b_R2(I, js):
            i = 4 * I + js
            rstd = sm2[:, 3:4]
            for c in range(8):
                T(lambda e, c=c: e.matmul(banks[4][:, 0:36], lhsT=hmT[:, c, :], rhs=Wr[:, c, :], start=(c == 0), stop=(c == 7)), [t_hmT, t_tab2], [tbank[4]], inc=(c == 7))
            V(lambda e: e.scalar_tensor_tensor(out=Lg_all[:, i, :], in0=banks[4][:, 0:36], scalar=rstd, in1=bias_b, op0=ALU.mult, op1=ALU.add), [tbank[4], t_smR, t_tab2], [t_L])

        n_st = NST if debug is None else debug.get("nst", NST)
        for js in range(4):
            b_load_a(0, js)
            b_load_b(0, js)
        for fc in range(8):
            b_qproj(0, fc)
        for I in range(n_st):
            nxt = I + 1 < n_st
            for hc in range(4):
                ks = [b_score(hc, 0), b_score(hc, 1)]
                if nxt:
                    b_load_a(I + 1, hc)
                for js in range(4):
                    b_pv(hc, js, ks)
                if nxt:
                    b_load_b(I + 1, hc)
            b_O1(I, 0)
            for js in range(4):
                b_O2(I, js)
                if js + 1 < 4:
                    b_O1(I, js + 1)
                if nxt:
                    b_qproj(I + 1, 2 * js, 4)
                b_R1(I, js)
                if nxt:
                    b_qproj(I + 1, 2 * js + 1, 4)
                b_R2(I, js)
            if I == 0:
                tap("octok", oc_tok, t_octok)

        if debug is not None and debug.get("stop") == "1B":
            P.barrier()
            finish_copy(P, A, out, hbuf, n_st * 4)
            P.build()
            return

        P.barrier()
        A.release(m_loop)
        N_ = NT
        rA = lambda n_: A.alloc([n_])
        gmax, gsum, m1, m2, dl, ex, rden, g1 = [rA(N_) for _ in range(8)]
        gm = A.alloc([N_, 4])
        gex = A.alloc([N_, 4])
        tmp48 = A.alloc([N_, 4, 8])
        esel, mk1, e2, mk2 = [A.alloc([N_, 8]) for _ in range(4)]
        M1 = A.alloc([N_, 4, 8])
        M2 = A.alloc([N_, 4, 8])
        Msum = A.alloc([N_, 32])
        Mb_all = A.alloc([N_ * 32], BF16)
        pit = A.alloc([N_, 32])
        totA = A.alloc([N_, 32])
        totB = A.alloc([N_, 32])
        tot0 = A.alloc([N_, 32])
        t_r = Trk()
        rw = [t_r]
        gl = Lg_all[:, :, 0:4]
        el = Lg_all[:, :, 4:36].rearrange("p i (g e) -> p i g e", g=4)
        V(lambda e: e.tensor_reduce(out=gmax, in_=gl, axis=AX.X, op=ALU.max), [t_L], rw)
        V(lambda e: e.tensor_tensor(out=gm, in0=gl, in1=bc(gmax, 2, [128, N_, 4]), op=ALU.is_equal), [t_L] + rw, rw)
        V(lambda e: e.tensor_tensor(out=gex, in0=gl, in1=bc(gmax, 2, [128, N_, 4]), op=ALU.subtract), [t_L] + rw, rw)
        Sc(lambda e: e.activation(out=gex, in_=gex, func=ACTF.Exp), rw, rw)
        V(lambda e: e.tensor_reduce(out=gsum, in_=gex, axis=AX.X, op=ALU.add), rw, rw)
        V(lambda e: e.tensor_tensor(out=tmp48, in0=el, in1=bc(gm, 3, [128, N_, 4, 8]), op=ALU.mult), [t_L] + rw, rw)
        V(lambda e: e.tensor_reduce(out=esel, in_=tmp48.rearrange("p i g e -> p i e g"), axis=AX.X, op=ALU.add), rw, rw)
        V(lambda e: e.tensor_reduce(out=m1, in_=esel, axis=AX.X, op=ALU.max), rw, rw)
        V(lambda e: e.tensor_tensor(out=mk1, in0=esel, in1=bc(m1, 2, [128, N_, 8]), op=ALU.is_equal), rw, rw)
        V(lambda e: e.scalar_tensor_tensor(out=e2, in0=mk1, scalar=-1e30, in1=esel, op0=ALU.mult, op1=ALU.add), rw, rw)
        V(lambda e: e.tensor_reduce(out=m2, in_=e2, axis=AX.X, op=ALU.max), rw, rw)
        V(lambda e: e.tensor_tensor(out=mk2, in0=e2, in1=bc(m2, 2, [128, N_, 8]), op=ALU.is_equal), rw, rw)
        V(lambda e: e.tensor_tensor(out=dl, in0=m2, in1=m1, op=ALU.subtract), rw, rw)
        Sc(lambda e: e.activation(out=ex, in_=dl, func=ACTF.Exp), rw, rw)
        V(lambda e: e.tensor_scalar(out=rden, in0=ex, scalar1=1.0, scalar2=None, op0=ALU.add), rw, rw)
        V(lambda e: e.tensor_tensor(out=rden, in0=rden, in1=gsum, op=ALU.mult), rw, rw)
        V(lambda e: e.reciprocal(out=g1, in_=rden), rw, rw)
        V(lambda e: e.tensor_copy(out=gates[:, :, 0], in_=g1), rw, [t_gd])
        V(lambda e: e.tensor_tensor(out=gates[:, :, 1], in0=g1, in1=ex, op=ALU.mult), rw, [t_gd])
        V(lambda e: e.tensor_tensor(out=M1, in0=bc(gm, 3, [128, N_, 4, 8]), in1=bc(mk1, 2, [128, N_, 4, 8]), op=ALU.mult), rw, rw)
        V(lambda e: e.tensor_tensor(out=M2, in0=bc(gm, 3, [128, N_, 4, 8]), in1=bc(mk2, 2, [128, N_, 4, 8]), op=ALU.mult), rw, rw)
        M1f = M1.rearrange("p i g e -> p i (g e)")
        M2f = M2.rearrange("p i g e -> p i (g e)")
        V(lambda e: e.tensor_tensor(out=Msum, in0=M1f, in1=M2f, op=ALU.add), rw, rw)
        V(lambda e: e.tensor_copy(out=Mb_all, in_=Msum.rearrange("p i e -> p (i e)")), rw, rw)
        for hf in range(2):
            T(lambda e, hf=hf: e.matmul(banks[hf][:, 0:512], lhsT=ltrib, rhs=Mb_all[:, hf * 512:(hf + 1) * 512], start=True, stop=True), rw + [t_c], [tbank[hf]])
            T(lambda e, hf=hf: e.matmul(banks[2 + hf][:, 0:512], lhsT=onesb, rhs=Mb_all[:, hf * 512:(hf + 1) * 512], start=True, stop=True), rw + [t_c], [tbank[2 + hf]])
            V(lambda e, hf=hf: e.tensor_copy(out=pit[:, hf * 16:(hf + 1) * 16, :], in_=banks[hf][:, 0:512].rearrange("p (i e) -> p i e", e=32)), [tbank[hf]], rw)
            V(lambda e, hf=hf: e.tensor_copy(out=tot0[:, hf * 16:(hf + 1) * 16, :], in_=banks[2 + hf][:, 0:512].rearrange("p (i e) -> p i e", e=32)), [tbank[2 + hf]], rw)
        V(lambda e: e.tensor_copy(out=totA, in_=tot0), rw, rw)
        sA, sB = totA, totB
        for sft in (1, 2, 4, 8, 16):
            V(lambda e, sA=sA, sB=sB, sft=sft: e.tensor_copy(out=sB[:, 0:sft, :], in_=sA[:, 0:sft, :]), rw, rw)
            V(lambda e, sA=sA, sB=sB, sft=sft: e.tensor_tensor(out=sB[:, sft:N_, :], in0=sA[:, sft:N_, :], in1=sA[:, 0:N_ - sft, :], op=ALU.add), rw, rw)
            sA, sB = sB, sA
        incl = sA
        V(lambda e: e.tensor_copy(out=cum, in_=incl[:, N_ - 1, :]), rw, [t_cum])
        V(lambda e: e.tensor_tensor(out=tot0, in0=incl, in1=tot0, op=ALU.subtract), rw, rw)
        V(lambda e: e.tensor_tensor(out=pit, in0=pit, in1=tot0, op=ALU.add), rw, rw)
        posv3 = posv.rearrange("p (i k) -> p i k", k=2)
        eidf3 = eidf.rearrange("p (i k) -> p i k", k=2)
        for k_, Mk in enumerate((M1f, M2f)):
            V(lambda e, Mk=Mk: e.tensor_tensor(out=Msum, in0=Mk, in1=pit, op=ALU.mult), rw, rw)
            V(lambda e, k_=k_: e.tensor_reduce(out=posv3[:, :, k_], in_=Msum, axis=AX.X, op=ALU.add), rw, [t_gd])
            V(lambda e, Mk=Mk: e.tensor_tensor(out=Msum, in0=Mk, in1=bc(iota_e, 1, [128, N_, 32]), op=ALU.mult), rw + [t_c], rw)
            V(lambda e, k_=k_: e.tensor_reduce(out=eidf3[:, :, k_], in_=Msum, axis=AX.X, op=ALU.add), rw, [t_gd])

        fz = A.alloc([2048])
        cA, cB = fz[:, 0:32], fz[:, 32:64]
        qf, qi_, corr = fz[:, 64:96], fz[:, 96:128].bitcast(I32), fz[:, 128:160]
        padded, pst = fz[:, 160:192], fz[:, 192:224]
        destf = fz[:, 256:320]
        bef = fz[:, 320:320 + NBLK]
        chf = fz[:, 416:416 + NBLK]
        big = A.alloc([NBLK, 32])
        t_fz = Trk()
        rwf = [t_fz]
        V(lambda e: e.tensor_scalar(out=cA, in0=cum, scalar1=float(US - 1), scalar2=1.0 / US, op0=ALU.add, op1=ALU.mult), [t_cum], rwf)
        V(lambda e: e.tensor_copy(out=qi_, in_=cA), rwf, rwf)
        V(lambda e: e.tensor_copy(out=qf, in_=qi_), rwf, rwf)
        V(lambda e: e.tensor_tensor(out=corr, in0=qf, in1=cA, op=ALU.is_gt), rwf, rwf)
        V(lambda e: e.tensor_tensor(out=qf, in0=qf, in1=corr, op=ALU.subtract), rwf, rwf)
        V(lambda e: e.tensor_scalar(out=padded, in0=qf, scalar1=float(US), scalar2=None, op0=ALU.mult), rwf, rwf)
        V(lambda e: e.tensor_copy(out=cA, in_=padded), rwf, rwf)
        src_, dst_ = cA, cB
        for sft in (1, 2, 4, 8, 16):
            V(lambda e, src_=src_, dst_=dst_, sft=sft: e.tensor_copy(out=dst_[:, 0:sft], in_=src_[:, 0:sft]), rwf, rwf)
            V(lambda e, src_=src_, dst_=dst_, sft=sft: e.tensor_tensor(out=dst_[:, sft:32], in0=src_[:, sft:32], in1=src_[:, 0:32 - sft], op=ALU.add), rwf, rwf)
            src_, dst_ = dst_, src_
        pends = src_
        V(lambda e: e.tensor_tensor(out=pst, in0=pends, in1=padded, op=ALU.subtract), rwf, rwf)
        b64 = big[:, 0:64, :]
        V(lambda e: e.tensor_tensor(out=b64, in0=bc(iota_e, 1, [128, 64, 32]), in1=bc(eidf, 2, [128, 64, 32]), op=ALU.is_equal), [t_gd, t_c], rwf)
        V(lambda e: e.tensor_tensor(out=b64, in0=b64, in1=bc(pst, 1, [128, 64, 32]), op=ALU.mult), rwf, rwf)
        V(lambda e: e.tensor_reduce(out=destf, in_=b64, axis=AX.X, op=ALU.add), rwf, rwf)
        V(lambda e: e.tensor_tensor(out=destf, in0=destf, in1=posv, op=ALU.add), rwf + [t_gd], rwf)
        V(lambda e: e.tensor_copy(out=dest, in_=destf), rwf, [t_gd])
        V(lambda e: e.tensor_tensor(out=big, in0=bc(pends, 1, [128, NBLK, 32]), in1=bc(cstt[:, C_BS:C_BS + NBLK], 2, [128, NBLK, 32]), op=ALU.is_le), rwf + [t_c], rwf)
        V(lambda e: e.tensor_reduce(out=bef, in_=big, axis=AX.X, op=ALU.add), rwf, rwf)
        V(lambda e: e.tensor_scalar(out=bef, in0=bef, scalar1=31.0, scalar2=None, op0=ALU.min), rwf, rwf)
        V(lambda e: e.memset(chf, 1.0), [], rwf)
        V(lambda e: e.tensor_tensor(out=chf[:, 2:NBLK], in0=bef[:, 2:NBLK], in1=bef[:, 0:NBLK - 2], op=ALU.not_equal), rwf, rwf)
        V(lambda e: e.tensor_copy(out=blkE, in_=bef), rwf, [t_gd])
        V(lambda e: e.tensor_scalar(out=bef, in0=bef, scalar1=128.0, scalar2=cstt[:, C_PI:C_PI + 1], op0=ALU.mult, op1=ALU.add), rwf + [t_c], rwf)
        V(lambda e: e.tensor_tensor(out=chf, in0=pends[:, 31:32].to_broadcast([128, NBLK]), in1=cstt[:, C_BS:C_BS + NBLK], op=ALU.is_le), rwf + [t_c], rwf)
        V(lambda e: e.scalar_tensor_tensor(out=bef, in0=chf, scalar=float(NE * 128), in1=bef, op0=ALU.mult, op1=ALU.add), rwf, rwf)
        V(lambda e: e.tensor_copy(out=widx, in_=bef), rwf, [t_gd])
        tap("dest", dest, t_gd)
        tap("blkE", blkE, t_gd)
        tap("widx", widx, t_gd)
        tap("cum", cum, t_cum)
        s_sc = P.sem("s_sc")

        def scatter(i, k_):
            P.op("gpsimd", lambda e: e.indirect_dma_start(out=xs_d, out_offset=bass.IndirectOffsetOnAxis(ap=dest[:, 2 * i + k_:2 * i + k_ + 1], axis=0), in_=F_all[:, i, :], in_offset=None),
                 reads=[t_F, t_gd], writes=[], sem=s_sc, amt=16)
        for i in range(n_st * 4):
            for k_ in range(2):
                scatter(i, k_)
        P.barrier()
        A.release(m_persist)

        NWB = 4
        Wall = [A.alloc([12288], BF16) for _ in range(NWB)]
        Wg = [w_[:, 0:4096].rearrange("p (c n) -> p c n", c=8) for w_ in Wall]
        Wu = [w_[:, 4096:8192].rearrange("p (c n) -> p c n", c=8) for w_ in Wall]
        Wd = [w_[:, 8192:12288].rearrange("p (c n) -> p c n", c=4) for w_ in Wall]
        t_wb = [Trk() for _ in range(NWB)]
        s_wb = [P.sem(f"s_wb{k_}") for k_ in range(NWB)]
        NXB = 6
        Xsb = [A.alloc([D], BF16) for _ in range(NXB)]
        t_xsb = [Trk() for _ in range(NXB)]
        s_xsb = [P.sem(f"s_xsb{k_}") for k_ in range(NXB)]
        XT = [A.alloc([8, 128], BF16) for _ in range(2)]
        t_XT = [Trk(), Trk()]
        gsb = A.alloc([512])
        hdn = A.alloc([512], BF16)
        hdnT = A.alloc([4, 128], BF16)
        ysb = [A.alloc([D]) for _ in range(2)]
        t_ysb = [Trk(), Trk()]
        s_y = [P.sem("s_y0"), P.sem("s_y1")]
        t_gsb, t_hdn, t_hdnT = Trk(), Trk(), Trk()
        NB = NBLK * UB if debug is None else debug.get("nblk", NBLK * UB)

        N_BCHK = 44

        def w_load(u):
            wb = u % NWB
            kw = dict(bounds_check=NE * 128 - 1, oob_is_err=False) if u >= NBLK - N_BCHK else {}
            P.op("gpsimd", lambda e: e.indirect_dma_start(out=Wall[wb], out_offset=None, in_=wbf,
                                                          in_offset=bass.IndirectOffsetOnAxis(ap=widx[:, u:u + 1], axis=0), **kw),
                 reads=[t_gd], writes=[t_wb[wb]], sem=s_wb[wb], amt=16)

        def X_load(b):
            xb = b % NXB
            P.dma("sync", Xsb[xb], xs_d[b * 128:(b + 1) * 128, :], s_xsb[xb], writes=[t_xsb[xb]])

        def S1(b):
            pb = b % 2
            xb = b % NXB
            tpv = bk_bf(0).rearrange("p (a b) -> p a b", b=128)
            for c in range(8):
                T(lambda e, c=c: e.transpose(tpv[:, c, :], Xsb[xb][:, c * 128:(c + 1) * 128], identb), [t_xsb[xb], t_c], [tbank[0]], inc=(c == 7))
            V(lambda e: e.tensor_copy(out=XT[pb], in_=tpv), [tbank[0]], [t_XT[pb]])

        def S2(b):
            pb = b % 2
            wb = (b // UB) % NWB
            for (bkk, W_) in ((2, Wg[wb]), (3, Wu[wb])):
                for c in range(8):
                    T(lambda e, c=c, bkk=bkk, W_=W_: e.matmul(banks[bkk][:, 0:512], lhsT=XT[pb][:, c, :], rhs=W_[:, c, :], start=(c == 0), stop=(c == 7)),
                      [t_XT[pb], t_wb[wb]], [tbank[bkk]], inc=(c == 7))
            Sc(lambda e: e.activation(out=gsb, in_=banks[2][:, 0:512], func=ACTF.Silu), [tbank[2]], [t_gsb])
            V(lambda e: e.tensor_tensor(out=hdn, in0=gsb, in1=banks[3][:, 0:512], op=ALU.mult), [t_gsb, tbank[3]], [t_hdn])

        def S3(b):
            tpv = bk_bf(1).rearrange("p (a b) -> p a b", b=128)
            for c in range(4):
                T(lambda e, c=c: e.transpose(tpv[:, c, :], hdn[:, c * 128:(c + 1) * 128], identb), [t_hdn, t_c], [tbank[1]], inc=(c == 3))
            Sc(lambda e: e.activation(out=hdnT, in_=tpv[:, 0:4, :], func=ACTF.Copy), [tbank[1]], [t_hdnT])

        def S4(b):
            pb = b % 2
            wb = (b // UB) % NWB
            for hf in range(2):
                bkk = 4 + hf
                for c in range(4):
                    T(lambda e, c=c, hf=hf, bkk=bkk: e.matmul(banks[bkk][:, 0:512], lhsT=hdnT[:, c, :], rhs=Wd[wb][:, c, hf * 512:(hf + 1) * 512], start=(c == 0), stop=(c == 3)),
                      [t_hdnT, t_wb[wb]], [tbank[bkk]], inc=(c == 3))
                if hf == 0:
                    Sc(lambda e: e.activation(out=ysb[pb][:, 0:512], in_=banks[4][:, 0:512], func=ACTF.Copy), [tbank[4]], [t_ysb[pb]])
                else:
                    V(lambda e: e.tensor_copy(out=ysb[pb][:, 512:1024], in_=banks[5][:, 0:512]), [tbank[5]], [t_ysb[pb]])
            P.dma("scalar", y_d[b * 128:(b + 1) * 128, :], ysb[pb], s_y[pb], reads=[t_ysb[pb]])

        NU = (NB + UB - 1) // UB
        for u0 in range(min(NWB, NU)):
            w_load(u0)
        for b0 in range(min(NXB, NB)):
            X_load(b0)
        S1(0)
        S2(0)
        if NB > 1:
            S1(1)
        for b in range(NB):
            S3(b)
            if b + 1 < NB:
                S2(b + 1)
            S4(b)
            if b % UB == UB - 1 and b // UB + NWB < NU:
                w_load(b // UB + NWB)
            if b + 2 < NB:
                S1(b + 2)
            if b + NXB < NB:
                X_load(b + NXB)
        P.barrier()
        A.release(m_persist)

        lnfin_b = A.alloc([D])
        t_lf = Trk()
        s_lf = P.sem("s_lf")
        P.dma("sync", lnfin_b, ln_final_w.partition_broadcast(128), s_lf, writes=[t_lf])
        N3 = 6
        hh = [A.alloc([D]) for _ in range(N3)]
        yg = [A.alloc([2, D]) for _ in range(N3)]
        ot = [A.alloc([D]) for _ in range(N3)]
        jk = A.alloc([D], BF16)
        sm3 = A.alloc([8])
        t_hh, t_yg, t_ot = [Trk() for _ in range(N3)], [[Trk(), Trk()] for _ in range(N3)], [Trk() for _ in range(N3)]
        s_hh = [P.sem(f"s_hh{k_}") for k_ in range(N3)]
        s_yg = [[P.sem(f"s_yg{k_}a"), P.sem(f"s_yg{k_}b")] for k_ in range(N3)]
        s_ot = [P.sem(f"s_ot{k_}") for k_ in range(N3)]
        t_jk, t_sm3 = Trk(), Trk()

        def fin_tile(i):
            pb = i % N3
            P.dma("sync", hh[pb], hbuf[i * 128:(i + 1) * 128, :], s_hh[pb], writes=[t_hh[pb]])
            for k_ in range(2):
                P.op("gpsimd", lambda e, k_=k_: e.indirect_dma_start(out=yg[pb][:, k_, :], out_offset=None, in_=y_d,
                                                                    in_offset=bass.IndirectOffsetOnAxis(ap=dest[:, 2 * i + k_:2 * i + k_ + 1], axis=0)),
                     reads=[t_gd], writes=[t_yg[pb][k_]], sem=s_yg[pb][k_], amt=16)
            acc = hh[pb]
            for k_ in range(2):
                V(lambda e, k_=k_: e.scalar_tensor_tensor(out=acc, in0=yg[pb][:, k_, :], scalar=gates[:, i, k_:k_ + 1], in1=acc, op0=ALU.mult, op1=ALU.add),
                  [t_yg[pb][k_], t_gd, t_hh[pb]], [t_hh[pb]])
            ss = sm3[:, 0:1]
            rstd = sm3[:, 1:2]
            Sc(lambda e: e.activation(out=jk, in_=acc, func=ACTF.Square, accum_out=ss), [t_hh[pb]], [t_jk, t_sm3])
            rstd_from_ss(ss, rstd, D, t_sm3)
            V(lambda e: e.scalar_tensor_tensor(out=ot[pb], in0=acc, scalar=rstd, in1=lnfin_b, op0=ALU.mult, op1=ALU.mult), [t_hh[pb], t_sm3, t_lf], [t_ot[pb]])
            P.dma("scalar", out[i * 128:(i + 1) * 128, :], ot[pb], s_ot[pb], reads=[t_ot[pb]])
        for i in range(n_st * 4):
            fin_tile(i)
        P.barrier()
        P.build()


def finish_copy(P, A, out, src, ntiles):
    buf = A.alloc([D])
    tb = Trk()
    s1 = P.sem("dbg_l")
    s2 = P.sem("dbg_s")
    ev = None
    for i in range(ntiles):
        P.dma("sync", buf, src[i * 128:(i + 1) * 128, :], s1, writes=[tb])
        ev = P.dma("sync", out[i * 128:(i + 1) * 128, :], buf, s2, reads=[tb])
    P.wait_events("sync", [(s2, s2.n, "dma")])


def make_consts():
    c = np.zeros((128, C_END), np.float32)
    k = np.arange(128)[:, None]
    q = np.arange(128)[None, :]
    c[:, C_ID:C_ID + 128] = np.eye(128, dtype=np.float32)
    c[:, C_CM:C_CM + 128] = np.where(k <= q, 0.0, NEG)
    c[:, C_PM:C_PM + 128] = np.where(k > q, 0.0, NEG)
    c[:, C_LT:C_LT + 128] = (k < q).astype(np.float32)
    c[:, C_ON:C_ON + 128] = 1.0
    inv = np.exp(-math.log(10000.0) * np.arange(0, 64, 2, dtype=np.float32) / 64).astype(np.float32)
    c[:, C_IF:C_IF + 32] = inv[None, :]
    c[:, C_IO:C_IO + 32] = np.arange(32, dtype=np.float32)[None, :]
    c[:, C_BS:C_BS + NBLK] = (np.arange(NBLK) * US).astype(np.float32)[None, :]
    c[:, C_PI] = np.arange(128, dtype=np.float32)
    return c


_W_NAMES = ["ln_mix_w", "w_in", "lambda_q1", "lambda_k1", "lambda_q2", "lambda_k2", "subln_w", "sinks", "w_out",
            "ln_cross_w", "ln_mem_w", "wq_cross", "wkv_cross", "wo_cross", "ln_moe_w", "w_group", "b_group",
            "w_expert", "b_expert", "w_gate", "w_up", "w_down"]


def make_in_maps(inputs, n=8):
    cst = make_consts()
    shared = {k: np.ascontiguousarray(np.asarray(inputs[k], dtype=np.float32)) for k in _W_NAMES}
    shared["ln_final_w"] = np.ascontiguousarray(np.asarray(inputs["ln_final_w"], dtype=np.float32).reshape(1, D))
    shared["cst"] = cst
    maps = []
    for b in range(n):
        m = dict(shared)
        m["x"] = np.ascontiguousarray(inputs["x"][b])
        m["mem"] = np.ascontiguousarray(inputs["mem"][b])
        m["post"] = np.ascontiguousarray(np.asarray(inputs["positions"][b], dtype=np.int32).reshape(NT, 128).T)
        maps.append(m)
    return maps


def kernel(**inputs):
    nc = bass.Bass("TRN2", target_bir_lowering=False)
    build_program(nc)
    maps = make_in_maps(inputs, 8)
    res = run_bass_kernel_spmd(nc, maps, core_ids=list(range(8)))
    return np.stack([np.asarray(r["out"]) for r in res.results], axis=0).astype(np.float32)
```
